# Optimizing a Trainium2 kernel written in Bass

```python
import math, functools
import jax, jax.numpy as jnp
from jax import lax
import numpy as np

D_MODEL = 1024
BATCH = 8
SEQ = 4096
DEPTH = 2

GRID_W = 64
CTX_LEN = 256
D_FF = 2816
DIFF_HEADS = 4
DIFF_HEAD_DIM = 64
DIFF_V_DIM = 2 * DIFF_HEAD_DIM
WIN_HEADS = 8
WIN_KV_HEADS = 2
WIN_HEAD_DIM = 64
WINDOW = 128
Q_BLOCK = 128
ROPE_BASE = 10000.0
ROPE_FREQS = DIFF_HEAD_DIM // 4
MLSTM_HEADS = 4
MLSTM_HEAD_DIM = 128
RET_HEADS = 4
RET_QK_DIM = 64
RET_V_DIM = 128
CHUNK = 128
ALPHA = (2.0 * DEPTH) ** 0.25
BETA = (8.0 * DEPTH) ** -0.25
N_EVEN = (DEPTH + 1) // 2
N_ODD = DEPTH // 2

ATTN_SIZES = (DIFF_HEADS * 2 * DIFF_HEAD_DIM, DIFF_HEADS * 2 * DIFF_HEAD_DIM, DIFF_HEADS * DIFF_V_DIM,
              WIN_HEADS * WIN_HEAD_DIM, WIN_KV_HEADS * WIN_HEAD_DIM, WIN_KV_HEADS * WIN_HEAD_DIM)
ATTN_IN = sum(ATTN_SIZES)
ATTN_OUT = DIFF_HEADS * DIFF_V_DIM + WIN_HEADS * WIN_HEAD_DIM
REC_SIZES = (MLSTM_HEADS * MLSTM_HEAD_DIM, MLSTM_HEADS * MLSTM_HEAD_DIM, MLSTM_HEADS * MLSTM_HEAD_DIM,
             MLSTM_HEADS * MLSTM_HEAD_DIM, 2 * 2 * MLSTM_HEADS,
             RET_HEADS * RET_QK_DIM, RET_HEADS * RET_QK_DIM, RET_HEADS * RET_V_DIM, RET_HEADS * RET_V_DIM)
REC_IN = sum(REC_SIZES)
REC_OUT = MLSTM_HEADS * MLSTM_HEAD_DIM + RET_HEADS * RET_V_DIM

kernel_name = 'hybrid_diffattn_swa_mlstm_retention_dit'


def layer_norm(x, g, b=None, eps=1e-5):
    xf = x.astype(jnp.float32)
    mu = xf.mean(-1, keepdims=True)
    var = jnp.square(xf - mu).mean(-1, keepdims=True)
    y = (xf - mu) * lax.rsqrt(var + eps) * g
    if b is not None:
        y = y + b
    return y.astype(x.dtype)


def rms_norm(x, g, eps=1e-5):
    xf = x.astype(jnp.float32)
    y = xf * lax.rsqrt(jnp.square(xf).mean(-1, keepdims=True) + eps) * g
    return y.astype(x.dtype)


def split_cols(p, sizes):
    out, start = [], 0
    for s in sizes:
        out.append(p[..., start:start + s])
        start += s
    return out


def modulation(cvec, w, b):
    return jax.nn.silu(cvec) @ w + b


def modulate(x, m, s):
    return x * (1.0 + m[:, :, s, 1]) + m[:, :, s, 0]


def post_norm_residual(x, y, m, s, weight, g, b):
    return layer_norm(ALPHA * x + weight * m[:, :, s, 2] * y, g, b)


def swiglu(h, w_in, w_out):
    gate, up = jnp.split(h @ w_in, 2, axis=-1)
    return (jax.nn.silu(gate) * up) @ w_out


def ffn_sublayer(stream, m, s, w_in, w_out, g, b):
    return post_norm_residual(stream, swiglu(modulate(stream, m, s), w_in, w_out), m, s, 0.5, g, b)


def axial_rope_tables(n_tokens):
    ROWS = n_tokens // GRID_W
    row = jnp.repeat(jnp.arange(ROWS), GRID_W)
    col = jnp.tile(jnp.arange(GRID_W), ROWS)
    inv = ROPE_BASE ** (-jnp.arange(ROPE_FREQS, dtype=jnp.float32) / ROPE_FREQS)
    ang = jnp.stack([row[:, None] * inv, col[:, None] * inv], axis=1)
    return jnp.cos(ang), jnp.sin(ang)


def apply_rope(x, cos, sin):
    mid = x.shape[2:-1]
    xs = x.reshape(*x.shape[:-1], 2, 2, ROPE_FREQS)
    x1, x2 = xs[..., 0, :], xs[..., 1, :]
    c = cos.reshape(cos.shape[0], *([1] * len(mid)), 2, ROPE_FREQS)
    s = sin.reshape(sin.shape[0], *([1] * len(mid)), 2, ROPE_FREQS)
    out = jnp.stack([x1 * c - x2 * s, x2 * c + x1 * s], axis=-2)
    return out.reshape(x.shape)


def to_blocks(a):
    B, T = a.shape[:2]
    return a.reshape(B, T // Q_BLOCK, Q_BLOCK, *a.shape[2:]).swapaxes(0, 1)


def from_blocks(a):
    nb, B, Q = a.shape[:3]
    return a.swapaxes(0, 1).reshape(B, nb * Q, *a.shape[3:])


def to_chunks(a):
    B, T = a.shape[:2]
    return jnp.moveaxis(a.reshape(B, T // CHUNK, CHUNK, *a.shape[2:]), (1, 3), (0, 2))


def from_chunks(y):
    y = jnp.moveaxis(y, (0, 2), (1, 3))
    return y.reshape(y.shape[0], y.shape[1] * y.shape[2], *y.shape[3:])


def diff_attention(q, k, v, lam):
    s = jnp.einsum('bqhid,bkhid->bhiqk', q, k).astype(jnp.float32) * (q.shape[-1] ** -0.5)
    p = jax.nn.softmax(s, axis=-1)
    a = p[:, :, 0] - lam * p[:, :, 1]
    return jnp.einsum('bhqk,bkhv->bqhv', a, v)


def sink_attention(q, keys, values, masks, sink):
    B, Q, H, d = q.shape
    KVH = keys[0].shape[2]
    G = H // KVH
    qg = q.reshape(B, Q, KVH, G, d) * (d ** -0.5)
    scores = []
    for k, m in zip(keys, masks):
        s = jnp.einsum('bqkgd,bskd->bkgqs', qg, k).astype(jnp.float32)
        scores.append(s if m is None else jnp.where(m, s, -jnp.inf))
    sink_col = jnp.broadcast_to(sink.astype(jnp.float32).reshape(1, KVH, G, 1, 1), (B, KVH, G, Q, 1))
    p = jax.nn.softmax(jnp.concatenate(scores + [sink_col], axis=-1), axis=-1)
    out, start = None, 0
    for v in values:
        n = v.shape[1]
        part = jnp.einsum('bkgqs,bskd->bqkgd', p[..., start:start + n], v)
        out = part if out is None else out + part
        start += n
    return out.reshape(B, Q, H, d)


def split_attn(p):
    B, T, _ = p.shape
    aq, ak, av, bq, bk, bv = split_cols(p, ATTN_SIZES)
    return (aq.reshape(B, T, DIFF_HEADS, 2, DIFF_HEAD_DIM), ak.reshape(B, T, DIFF_HEADS, 2, DIFF_HEAD_DIM),
            av.reshape(B, T, DIFF_HEADS, DIFF_V_DIM), bq.reshape(B, T, WIN_HEADS, WIN_HEAD_DIM),
            bk.reshape(B, T, WIN_KV_HEADS, WIN_HEAD_DIM), bv.reshape(B, T, WIN_KV_HEADS, WIN_HEAD_DIM))


def attention_mixer(hx, hc, w_in, w_out, lam_vec, sub_g, sink, lambda_init, need_ctx_out):
    B, T, _ = hx.shape
    cos, sin = axial_rope_tables(T)
    aqx, akx, avx, bqx, bkx, bvx = split_attn(hx @ w_in)
    aqc, akc, avc, bqc, bkc, bvc = split_attn(hc @ w_in)
    lam = (jnp.exp(jnp.sum(lam_vec[0] * lam_vec[1])) - jnp.exp(jnp.sum(lam_vec[2] * lam_vec[3]))
           + lambda_init).astype(jnp.float32)
    k_all = jnp.concatenate([apply_rope(akx, cos, sin), akc], axis=1)
    v_all = jnp.concatenate([avx, avc], axis=1)
    a_x = from_blocks(lax.map(lambda qb: diff_attention(qb, k_all, v_all, lam),
                              to_blocks(apply_rope(aqx, cos, sin))))
    pad = ((0, 0), (WINDOW, WINDOW), (0, 0), (0, 0))
    k_pad = jnp.pad(apply_rope(bkx, cos, sin), pad)
    v_pad = jnp.pad(bvx, pad)
    span = Q_BLOCK + 2 * WINDOW
    q_pos = jnp.arange(Q_BLOCK)
    k_off = jnp.arange(span) - WINDOW

    def window_block(args):
        i, qb = args
        start = i * Q_BLOCK
        kw = lax.dynamic_slice_in_dim(k_pad, start, span, axis=1)
        vw = lax.dynamic_slice_in_dim(v_pad, start, span, axis=1)
        j = start + k_off
        t = start + q_pos
        mask = (j[None, :] >= 0) & (j[None, :] < T) & (jnp.abs(t[:, None] - j[None, :]) <= WINDOW)
        return sink_attention(qb, (kw, bkc), (vw, bvc), (mask, None), sink)

    b_x = from_blocks(lax.map(window_block, (jnp.arange(T // Q_BLOCK), to_blocks(apply_rope(bqx, cos, sin)))))

    def merge(a, b_out):
        Bm, Tm = a.shape[:2]
        a = rms_norm(a, sub_g) * (1.0 - lambda_init)
        return jnp.concatenate([a.reshape(Bm, Tm, -1), b_out.reshape(Bm, Tm, -1)], axis=-1) @ w_out

    y_x = merge(a_x, b_x)
    y_c = None
    if need_ctx_out:
        a_c = diff_attention(aqc, akc, avc, lam)
        b_c = sink_attention(bqc, (bkc,), (bvc,), (None,), sink)
        y_c = merge(a_c, b_c)
    return y_x, y_c


def mlstm_scan(seqs, state, with_output):
    q, k, v, i_pre, log_f = seqs
    tril = jnp.tril(jnp.ones((CHUNK, CHUNK), dtype=bool))

    def step(carry, xs):
        C, n, m = carry
        qc, kc, vc, ic, fc = xs
        b = jnp.cumsum(fc, axis=-1)
        b_end = b[..., -1]
        w_end = b_end[..., None] - b + ic
        m_new = jnp.maximum(b_end + m, w_end.max(-1))
        decay_state = jnp.exp(b_end + m - m_new)
        w = jnp.exp(w_end - m_new[..., None])
        C_new = decay_state[..., None, None] * C + jnp.einsum('bhl,bhlk,bhlv->bhkv', w, kc, vc)
        n_new = decay_state[..., None] * n + jnp.einsum('bhl,bhlk->bhk', w, kc)
        if not with_output:
            return (C_new, n_new, m_new), None
        log_d = jnp.where(tril, b[..., :, None] - b[..., None, :] + ic[..., None, :], -jnp.inf)
        log_inter = b + m[..., None]
        m_t = jnp.maximum(log_inter, log_d.max(-1))
        s = jnp.einsum('bhqd,bhkd->bhqk', qc, kc) * jnp.exp(log_d - m_t[..., None])
        w_inter = jnp.exp(log_inter - m_t)
        num = jnp.einsum('bhqk,bhkv->bhqv', s, vc) + w_inter[..., None] * jnp.einsum('bhqd,bhdv->bhqv', qc, C)
        den = s.sum(-1) + w_inter * jnp.einsum('bhqd,bhd->bhq', qc, n)
        h = num / jnp.maximum(jnp.abs(den), jnp.exp(-m_t))[..., None]
        return (C_new, n_new, m_new), h

    state, ys = lax.scan(step, state, tuple(to_chunks(a) for a in seqs))
    return (from_chunks(ys) if with_output else None), state


def retention_scan(log_gamma, seqs, state, with_output):
    q, k, v = seqs
    pos = jnp.arange(CHUNK, dtype=jnp.float32)
    g = log_gamma[:, None]
    decay_to_end = jnp.exp(g * (CHUNK - 1 - pos))
    decay_chunk = jnp.exp(log_gamma * CHUNK)
    decay_from_start = jnp.exp(g * (pos + 1.0))
    rel = pos[:, None] - pos[None, :]
    decay_intra = jnp.where(rel >= 0, jnp.exp(log_gamma[:, None, None] * jnp.maximum(rel, 0.0)), 0.0)

    def step(S, xs):
        qc, kc, vc = xs
        S_new = decay_chunk[None, :, None, None] * S + jnp.einsum('hl,bhlk,bhlv->bhkv', decay_to_end, kc, vc)
        if not with_output:
            return S_new, None
        s = jnp.einsum('bhqd,bhkd->bhqk', qc, kc) * decay_intra
        o = (jnp.einsum('bhqk,bhkv->bhqv', s, vc)
             + decay_from_start[None, :, :, None] * jnp.einsum('bhqd,bhdv->bhqv', qc, S))
        return S_new, o

    state, ys = lax.scan(step, state, tuple(to_chunks(a) for a in seqs))
    return (from_chunks(ys) if with_output else None), state


def run_bidirectional(scans, lat_seqs, ctx_seqs, state0, need_ctx_out):
    lat_outs, ctx_outs = [], []
    for d in range(2):
        flip = (lambda a: jnp.flip(a, axis=1)) if d == 1 else (lambda a: a)
        ctx_h, ctx_state = scans[d](tuple(flip(a) for a in ctx_seqs[d]), state0, need_ctx_out)
        lat_h, _ = scans[d](tuple(flip(a) for a in lat_seqs[d]), ctx_state, True)
        lat_outs.append(flip(lat_h))
        if need_ctx_out:
            ctx_outs.append(flip(ctx_h))
    ctx_sum = ctx_outs[0] + ctx_outs[1] if need_ctx_out else None
    return lat_outs[0] + lat_outs[1], ctx_sum


def recurrent_mixer(hx, hc, w_in, w_out, gate_b, m_norm_g, ret_logit, ret_g, ret_b, need_ctx_out):
    H, d = MLSTM_HEADS, MLSTM_HEAD_DIM

    def prep(p):
        B, T, _ = p.shape
        mq, mk, mv, mo, mg, rq, rk, rv, rg = split_cols(p, REC_SIZES)
        gates = (mg.reshape(B, T, 2, 2, H) + gate_b).astype(jnp.float32)
        q, k, v = mq.reshape(B, T, H, d), mk.reshape(B, T, H, d) * (d ** -0.5), mv.reshape(B, T, H, d)
        m_seqs = [(q, k, v, gates[:, :, dr, 0], jax.nn.log_sigmoid(gates[:, :, dr, 1])) for dr in range(2)]
        r_qkv = (rq.reshape(B, T, RET_HEADS, RET_QK_DIM),
                 rk.reshape(B, T, RET_HEADS, RET_QK_DIM) * (RET_QK_DIM ** -0.5),
                 rv.reshape(B, T, RET_HEADS, RET_V_DIM))
        return m_seqs, [r_qkv, r_qkv], mo, rg

    m_x, r_x, o_x, g_x = prep(hx @ w_in)
    m_c, r_c, o_c, g_c = prep(hc @ w_in)
    B = hx.shape[0]
    m_state0 = (jnp.zeros((B, H, d, d), jnp.float32), jnp.zeros((B, H, d), jnp.float32),
                jnp.zeros((B, H), jnp.float32))
    r_state0 = jnp.zeros((B, RET_HEADS, RET_QK_DIM, RET_V_DIM), jnp.float32)
    log_gamma = jax.nn.log_sigmoid(ret_logit.astype(jnp.float32))
    m_lat, m_ctx = run_bidirectional([mlstm_scan, mlstm_scan], m_x, m_c, m_state0, need_ctx_out)
    r_scans = [functools.partial(retention_scan, log_gamma[dr]) for dr in range(2)]
    r_lat, r_ctx = run_bidirectional(r_scans, r_x, r_c, r_state0, need_ctx_out)

    def merge(h_m, h_r, o, g):
        Bm, Tm = o.shape[:2]
        hm = layer_norm(h_m, m_norm_g.reshape(H, d)) * jax.nn.sigmoid(o).reshape(Bm, Tm, H, d)
        hr = (layer_norm(h_r, ret_g.reshape(RET_HEADS, RET_V_DIM), ret_b.reshape(RET_HEADS, RET_V_DIM))
              * jax.nn.silu(g).reshape(Bm, Tm, RET_HEADS, RET_V_DIM))
        return jnp.concatenate([hm.reshape(Bm, Tm, -1), hr.reshape(Bm, Tm, -1)], axis=-1) @ w_out

    y_x = merge(m_lat, r_lat, o_x, g_x)
    y_c = merge(m_ctx, r_ctx, o_c, g_c) if need_ctx_out else None
    return y_x, y_c


def setup_inputs(seed: int = 0) -> dict:
    key = jax.random.key(seed)
    ks = jax.random.split(key, 22)
    nrm = lambda k, shape, s: jax.random.normal(k, shape, jnp.float32) * s
    gate_base = jnp.stack([jnp.zeros((MLSTM_HEADS,), jnp.float32), jnp.linspace(3.0, 6.0, MLSTM_HEADS)])
    gamma0 = 1.0 - 2.0 ** (-5.0 - jnp.arange(RET_HEADS, dtype=jnp.float32))
    return {
        'x': nrm(ks[0], (BATCH, SEQ, D_MODEL), 1.0),
        'c': nrm(ks[1], (BATCH, D_MODEL), 1.0),
        'ctx': nrm(ks[2], (BATCH, CTX_LEN, D_MODEL), 1.0),
        'c_ctx': nrm(ks[3], (D_MODEL,), 1.0),
        'ada_w': nrm(ks[4], (DEPTH, D_MODEL, 9 * D_MODEL), D_MODEL ** -0.5),
        'ada_b': nrm(ks[5], (DEPTH, 9 * D_MODEL), 0.02),
        'ln_g': 1.0 + nrm(ks[6], (DEPTH, 3, D_MODEL), 0.02),
        'ln_b': nrm(ks[7], (DEPTH, 3, D_MODEL), 0.02),
        'ffn_w_in': nrm(ks[8], (DEPTH, 2, D_MODEL, 2 * D_FF), D_MODEL ** -0.5),
        'ffn_w_out': nrm(ks[9], (DEPTH, 2, D_FF, D_MODEL), BETA * D_FF ** -0.5),
        'attn_w_in': nrm(ks[10], (N_EVEN, D_MODEL, ATTN_IN), D_MODEL ** -0.5),
        'attn_w_out': nrm(ks[11], (N_EVEN, ATTN_OUT, D_MODEL), BETA * ATTN_OUT ** -0.5),
        'diff_lambda': nrm(ks[12], (N_EVEN, 4, DIFF_HEAD_DIM), 0.1),
        'diff_norm_g': 1.0 + nrm(ks[13], (N_EVEN, DIFF_V_DIM), 0.02),
        'sink_logits': nrm(ks[14], (N_EVEN, WIN_HEADS), 0.5),
        'rec_w_in': nrm(ks[15], (N_ODD, D_MODEL, REC_IN), D_MODEL ** -0.5),
        'rec_w_out': nrm(ks[16], (N_ODD, REC_OUT, D_MODEL), BETA * REC_OUT ** -0.5),
        'mlstm_gate_b': gate_base + nrm(ks[17], (N_ODD, 2, 2, MLSTM_HEADS), 0.1),
        'mlstm_norm_g': 1.0 + nrm(ks[18], (N_ODD, MLSTM_HEADS * MLSTM_HEAD_DIM), 0.02),
        'ret_decay_logit': jnp.log(gamma0 / (1.0 - gamma0)) + nrm(ks[19], (N_ODD, 2, RET_HEADS), 0.1),
        'ret_norm_g': 1.0 + nrm(ks[20], (N_ODD, RET_HEADS * RET_V_DIM), 0.02),
        'ret_norm_b': nrm(ks[21], (N_ODD, RET_HEADS * RET_V_DIM), 0.02),
    }


def reference(x, c, ctx, c_ctx, ada_w, ada_b, ln_g, ln_b, ffn_w_in, ffn_w_out, attn_w_in, attn_w_out,
              diff_lambda, diff_norm_g, sink_logits, rec_w_in, rec_w_out, mlstm_gate_b, mlstm_norm_g,
              ret_decay_logit, ret_norm_g, ret_norm_b):
    out_dtype = x.dtype
    B, D = x.shape[0], x.shape[2]
    for l in range(DEPTH):
        last = l == DEPTH - 1
        mod_x = modulation(c, ada_w[l], ada_b[l]).reshape(B, 1, 3, 3, D)
        mod_c = modulation(c_ctx, ada_w[l], ada_b[l]).reshape(1, 1, 3, 3, D)
        x = ffn_sublayer(x, mod_x, 0, ffn_w_in[l, 0], ffn_w_out[l, 0], ln_g[l, 0], ln_b[l, 0])
        ctx = ffn_sublayer(ctx, mod_c, 0, ffn_w_in[l, 0], ffn_w_out[l, 0], ln_g[l, 0], ln_b[l, 0])
        hx, hc = modulate(x, mod_x, 1), modulate(ctx, mod_c, 1)
        i = l // 2
        if l % 2 == 0:
            lambda_init = 0.8 - 0.6 * math.exp(-0.3 * l)
            y_x, y_c = attention_mixer(hx, hc, attn_w_in[i], attn_w_out[i], diff_lambda[i], diff_norm_g[i],
                                       sink_logits[i], lambda_init, not last)
        else:
            y_x, y_c = recurrent_mixer(hx, hc, rec_w_in[i], rec_w_out[i], mlstm_gate_b[i], mlstm_norm_g[i],
                                       ret_decay_logit[i], ret_norm_g[i], ret_norm_b[i], not last)
        x = post_norm_residual(x, y_x, mod_x, 1, 1.0, ln_g[l, 1], ln_b[l, 1])
        x = ffn_sublayer(x, mod_x, 2, ffn_w_in[l, 1], ffn_w_out[l, 1], ln_g[l, 2], ln_b[l, 2])
        if not last:
            ctx = post_norm_residual(ctx, y_c, mod_c, 1, 1.0, ln_g[l, 1], ln_b[l, 1])
            ctx = ffn_sublayer(ctx, mod_c, 2, ffn_w_in[l, 1], ffn_w_out[l, 1], ln_g[l, 2], ln_b[l, 2])
    return x.astype(out_dtype)
```

```python
import math
import os
from contextlib import ExitStack
import numpy as np
import concourse.bass as bass
import concourse.mybir as mybir
from concourse.bass_utils import run_bass_kernel_spmd

F32 = mybir.dt.float32
BF16 = mybir.dt.bfloat16
ALU = mybir.AluOpType
AF = mybir.ActivationFunctionType

D = 1024
T = 4096
TC = 256
NT = (T + TC) // 128
NTX = T // 128
DFF = 2816
NJ = DFF // 128
DEPTH = 2
ALPHA = (2.0 * DEPTH) ** 0.25
EPS = 1e-5
ATTN_IN = 2304
REC_IN = 3600


class Buf:
    __slots__ = ("name", "w", "r")

    def __init__(self, name=""):
        self.name = name
        self.w = None
        self.r = {}


class Sched:
    ENG = ("pe", "act", "dve", "pool", "sp")

    def __init__(self, nc, es, n_dma_sems=10):
        self.nc = nc
        self.engs = {"pe": nc.tensor, "act": nc.scalar, "dve": nc.vector, "pool": nc.gpsimd, "sp": nc.sync}
        self.sems = []
        self.cnt = []
        self.esem = {}
        for e in self.ENG:
            self.esem[e] = self._newsem(es, "c_" + e)
        self.dsem = {q: [self._newsem(es, "d_%s%d" % (q, i)) for i in range(n_dma_sems)] for q in ("sp", "pool")}
        self.dptr = {q: 0 for q in self.dsem}
        self.seen = {e: {} for e in self.ENG}
        self.nwait = 0
        self.nins = 0
        self.log = {e: [] for e in self.ENG}

    def _newsem(self, es, name):
        self.sems.append(es.enter_context(self.nc.semaphore(name)))
        self.cnt.append(0)
        return len(self.sems) - 1

    def _waits(self, e, reads, writes):
        need = {}
        own = self.esem[e]
        for b in reads:
            if b.w is not None:
                s, v = b.w
                if need.get(s, 0) < v:
                    need[s] = v
        for b in writes:
            if b.w is not None:
                s, v = b.w
                if s != own and need.get(s, 0) < v:
                    need[s] = v
            for s, v in b.r.items():
                if s != own and need.get(s, 0) < v:
                    need[s] = v
        seen = self.seen[e]
        eng = self.engs[e]
        for s, v in need.items():
            if seen.get(s, 0) >= v:
                continue
            eng.wait_ge(self.sems[s], v)
            self.log[e].append(("w", s, v))
            self.nwait += 1
            seen[s] = v

    def _mark(self, tok, reads, writes):
        s, v = tok
        for b in reads:
            if b.r.get(s, 0) < v:
                b.r[s] = v
        for b in writes:
            b.w = tok
            b.r = {}

    def op(self, e, fn, reads=(), writes=(), inc=True):
        self._waits(e, reads, writes)
        ins = fn(self.engs[e])
        self.nins += 1
        s = self.esem[e]
        if inc:
            self.cnt[s] += 1
            ins.then_inc(self.sems[s], 1)
            tok = (s, self.cnt[s])
            self.log[e].append(("i", s, 1))
        else:
            tok = (s, self.cnt[s] + 1)
        self._mark(tok, reads, writes)
        return tok

    def dma(self, q, out, in_, reads=(), writes=(), **kw):
        lst = self.dsem[q]
        i = self.dptr[q]
        self.dptr[q] = (i + 1) % len(lst)
        s = lst[i]
        eng = self.engs[q]
        if self.cnt[s] > 0 and self.seen[q].get(s, 0) < self.cnt[s]:
            eng.wait_ge(self.sems[s], self.cnt[s])
            self.log[q].append(("w", s, self.cnt[s]))
            self.seen[q][s] = self.cnt[s]
        self._waits(q, reads, writes)
        ins = eng.dma_start(out=out, in_=in_, **kw)
        self.nins += 1
        self.cnt[s] += 16
        ins.then_inc(self.sems[s], 16)
        self.log[q].append(("i", s, 16))
        tok = (s, self.cnt[s])
        self._mark(tok, reads, writes)
        return tok

    def barrier(self):
        for e in self.ENG:
            eng = self.engs[e]
            seen = self.seen[e]
            for s in range(len(self.sems)):
                v = self.cnt[s]
                if v > 0 and seen.get(s, 0) < v and s != self.esem[e]:
                    eng.wait_ge(self.sems[s], v)
                    self.log[e].append(("w", s, v))
                    seen[s] = v

    def finish(self):
        eng = self.engs["sp"]
        seen = self.seen["sp"]
        for s in range(len(self.sems)):
            v = self.cnt[s]
            if v > 0 and seen.get(s, 0) < v:
                eng.wait_ge(self.sems[s], v)
                self.log["sp"].append(("w", s, v))
                seen[s] = v

    def check_deadlock(self):
        val = [0] * len(self.sems)
        pos = {e: 0 for e in self.ENG}
        progress = True
        while progress:
            progress = False
            for e in self.ENG:
                lg = self.log[e]
                i = pos[e]
                while i < len(lg):
                    kind, s, v = lg[i]
                    if kind == "w":
                        if val[s] < v:
                            break
                    else:
                        val[s] += v
                    i += 1
                if i != pos[e]:
                    progress = True
                    pos[e] = i
        stuck = {e: (pos[e], len(self.log[e]), self.log[e][pos[e]]) for e in self.ENG if pos[e] < len(self.log[e])}
        return stuck


class K:
    pass


_UID = [0]


def _sb(k, es, name, shape, dt):
    _UID[0] += 1
    t = es.enter_context(k.nc.sbuf_tensor("%s_%d" % (name, _UID[0]), list(shape), dt))
    return t


def build_nc(stop_after=None):
    nc = bass.Bass("TRN2", target_bir_lowering=False)
    k = K()
    k.nc = nc
    dram = lambda name, shape, dt=F32, kind="ExternalInput": nc.dram_tensor(name, list(shape), dt, kind=kind).ap()
    k.x_in = dram("x", [T, D])
    k.ctx_in = dram("ctx", [TC, D])
    k.cc_in = dram("cc", [16, 128])
    k.ada_w = dram("ada_w", [DEPTH, D, 9 * D])
    k.ada_b = dram("ada_b", [DEPTH, 9 * D])
    k.ln_g = dram("ln_g", [DEPTH, 3, D])
    k.ln_b = dram("ln_b", [DEPTH, 3, D])
    k.ffn_w_in = dram("ffn_w_in", [DEPTH, 2, D, 2 * DFF])
    k.ffn_w_out = dram("ffn_w_out", [DEPTH, 2, DFF, D])
    k.ident_in = dram("ident", [128, 128])
    k.attn_w_in = dram("attn_w_in", [1, D, ATTN_IN])
    k.attn_w_out = dram("attn_w_out", [1, D, D])
    k.diff_lambda = dram("diff_lambda", [1, 4, 64])
    k.diff_norm_g = dram("diff_norm_g", [1, 128])
    k.sink_logits = dram("sink_logits", [1, 8])
    k.rec_w_in = dram("rec_w_in", [1, D, REC_IN])
    k.rec_w_out = dram("rec_w_out", [1, D, D])
    k.mlstm_gate_b = dram("mlstm_gate_b", [1, 2, 2, 4])
    k.mlstm_norm_g = dram("mlstm_norm_g", [1, 512])
    k.ret_decay_logit = dram("ret_decay_logit", [1, 2, 4])
    k.ret_norm_g = dram("ret_norm_g", [1, 512])
    k.ret_norm_b = dram("ret_norm_b", [1, 512])
    k.tri_in = dram("tri", [2, 128, 128])
    k.mb_in = dram("mbm", [2, 128, 128])
    k.mw_in = dram("mwm", [2, 128, 128])
    k.mall_in = dram("mall", [128, 128])
    k.pos_in = dram("posc", [128, 8])
    k.perm_in = dram("perm", [128, 128])
    k.maskp_in = dram("maskp", [128, 512])
    k.maskn_in = dram("maskn", [128, 512])
    k.ropec_in = dram("ropec", [128, T])
    k.ropes_in = dram("ropes", [128, T])
    k.out = dram("out", [T, D], kind="ExternalOutput")
    k.dbg_on = bool(os.environ.get("KDBG"))
    if k.dbg_on:
        k.dbg = dram("dbg", [NT * 128, D], kind="ExternalOutput")
        k.BBd = nc.dram_tensor("BBd", [NT * 128, 512], F32).ap()
        k.BBb = [Buf() for _ in range(NT)]
    k.X = nc.dram_tensor("Xs", [NT * 128, D], F32).ap()
    k.MODROW = nc.dram_tensor("modrow", [DEPTH, 2, 9 * D], F32).ap()
    k.Xb = [Buf("X%d" % t) for t in range(NT)]
    k.ABd = nc.dram_tensor("ABd", [NT * 128, 512], F32).ap()
    k.ABb = [Buf("AB%d" % t) for t in range(NT)]
    k.H0d = nc.dram_tensor("H0d", [T, D], F32).ap()
    k.H0b = [Buf("H0%d" % t) for t in range(NTX)]
    k.modrow_b = [Buf("modrow%d" % l) for l in range(DEPTH)]
    k.stop_after = stop_after

    with ExitStack() as es:
        S = Sched(nc, es)
        k.S = S
        k.ps = []
        k.psb = []
        for i in range(8):
            k.ps.append(es.enter_context(nc.psum_tensor("ps%d" % i, [128, 512], F32)))
            k.psb.append(Buf("ps%d" % i))
        k.ident = _sb(k, es, "ident", [128, 128], F32)
        k.ident_b = Buf("ident")
        S.dma("sp", k.ident[:], k.ident_in[:, :], writes=[k.ident_b])
        emit_program(k)
        S.finish()
        stuck = S.check_deadlock()
        print("instructions", S.nins, "waits", S.nwait, "sem counts", S.cnt[:5], "DEADLOCK" if stuck else "no-deadlock", stuck, flush=True)
    return nc


def emit_program(k):
    phase_mod(k)
    src0 = lambda t: (k.x_in[t * 128:(t + 1) * 128, :] if t < NTX else k.ctx_in[(t - NTX) * 128:(t - NTX + 1) * 128, :])
    phase_ffn(k, 0, 0, 0, src0, NT)
    if k.stop_after == "ffn00":
        return copy_out(k)
    with ExitStack() as es:
        load_attn_consts(k, es)
        if k.stop_after == "attn_c":
            k.S.barrier()
            return copy_out(k)
        phase_attn_a(k)
        if k.stop_after in ("attn_a", "attn_ai"):
            return copy_out(k)
        phase_attn_b(k)
    if k.stop_after == "mix0":
        return copy_out(k)
    srcX = lambda t: k.X[t * 128:(t + 1) * 128, :]
    phase_ffn(k, 0, 1, 2, srcX, NT)
    if k.stop_after == "ffn01":
        return copy_out(k)
    phase_ffn(k, 1, 0, 0, srcX, NT)
    if k.stop_after == "ffn10":
        return copy_out(k)
    with ExitStack() as es:
        load_rec_consts(k, es)
        if k.stop_after == "rec_c":
            k.S.barrier()
            return copy_out(k)
        phase_rec(k, 0)
        if k.stop_after == "rec0":
            return copy_out(k)
        phase_rec(k, 1)
    if k.stop_after == "mix1":
        return copy_out(k)
    phase_ffn(k, 1, 1, 2, srcX, NTX)
    copy_out(k)


def copy_out(k):
    S = k.S
    if k.dbg_on:
        for t in range(NT):
            S.dma("sp", k.dbg[t * 128:(t + 1) * 128, 0:512], k.ABd[t * 128:(t + 1) * 128, :], reads=[k.ABb[t]])
            S.dma("sp", k.dbg[t * 128:(t + 1) * 128, 512:1024], k.BBd[t * 128:(t + 1) * 128, :], reads=[k.BBb[t]])
    for t in range(0, NTX, 4):
        S.dma("sp", k.out[t * 128:(t + 4) * 128, :], k.X[t * 128:(t + 4) * 128, :],
              reads=[k.Xb[t + i] for i in range(4)])


def phase_mod(k):
    nc, S = k.nc, k.S
    with ExitStack() as es:
        cc = _sb(k, es, "cc", [16, 128], F32)
        ccs = _sb(k, es, "ccs", [16, 128], F32)
        scb = _sb(k, es, "scb", [128, 16], BF16)
        brow = _sb(k, es, "brow", [2, 9 * D], F32)
        mrow = _sb(k, es, "mrow", [2, 9 * D], F32)
        wb = [_sb(k, es, "wblk%d" % i, [128, 8, 512], BF16) for i in range(2)]
        b_cc, b_ccs, b_scb, b_brow, b_mrow = Buf(), Buf(), Buf(), Buf(), Buf()
        b_wb = [Buf(), Buf()]
        S.dma("sp", cc[:], k.cc_in[:, :], writes=[b_cc])
        S.op("act", lambda e: e.activation(out=ccs[:], in_=cc[:], func=AF.Silu), reads=[b_cc], writes=[b_ccs])
        tp = k.ps[6]
        S.op("pe", lambda e: e.transpose(out=tp[:, 0:16], in_=ccs[:], identity=k.ident[0:16, 0:16]),
             reads=[b_ccs, k.ident_b], writes=[k.psb[6]])
        S.op("dve", lambda e: e.tensor_copy(out=scb[:], in_=tp[:, 0:16]), reads=[k.psb[6]], writes=[b_scb])
        scb3 = scb[:].rearrange("p (v c) -> p v c", v=2)
        blk = 0
        for l in range(DEPTH):
            S.dma("sp", brow[:], k.ada_b[l, :].partition_broadcast(2), writes=[b_brow])
            wsrc = k.ada_w[l].rearrange("(c p) n -> p c n", p=128)
            for nb in range(18):
                w = wb[blk % 2]
                bw = b_wb[blk % 2]
                S.dma("pool", w[:], wsrc[:, :, nb * 512:(nb + 1) * 512], writes=[bw])
                pb = 4 + (blk % 2)
                for kc in range(8):
                    S.op("pe", lambda e, kc=kc, w=w, pb=pb: e.matmul(k.ps[pb][0:2, :], lhsT=scb3[:, :, kc], rhs=w[:, kc, :],
                                                                    start=(kc == 0), stop=(kc == 7)),
                         reads=[b_scb, bw], writes=[k.psb[pb]], inc=(kc == 7))
                S.op("dve", lambda e, pb=pb, nb=nb: e.tensor_tensor(out=mrow[:, nb * 512:(nb + 1) * 512], in0=k.ps[pb][0:2, :],
                                                                   in1=brow[:, nb * 512:(nb + 1) * 512], op=ALU.add),
                     reads=[k.psb[pb], b_brow], writes=[b_mrow])
                blk += 1
            S.dma("sp", k.MODROW[l], mrow[:], reads=[b_mrow], writes=[k.modrow_b[l]])
        S.barrier()


def load_mod(k, es, l, s, tag):
    nc, S = k.nc, k.S
    m = K()
    m.st = _sb(k, es, "mst" + tag, [32, 128], F32)
    m.mc = _sb(k, es, "mc" + tag, [128, 32], F32)
    m.gb = [_sb(k, es, "gb%d" % v + tag, [128, D], F32) for v in range(2)]
    m.lng = _sb(k, es, "lng" + tag, [128, D], F32)
    m.lnb = _sb(k, es, "lnb" + tag, [128, D], F32)
    m.b_st, m.b_mc, m.b_gb, m.b_ln = Buf(), Buf(), Buf(), Buf()
    for j in range(2):
        for v in range(2):
            r0 = (j * 2 + v) * 8
            src = k.MODROW[l, v, (s * 3 + j) * D:(s * 3 + j + 1) * D].rearrange("(c p) -> c p", p=128)
            S.dma("sp", m.st[r0:r0 + 8, :], src, reads=[k.modrow_b[l]], writes=[m.b_st])
    tp = k.ps[6]
    S.op("pe", lambda e: e.transpose(out=tp[:, 0:32], in_=m.st[:], identity=k.ident[0:32, 0:32]),
         reads=[m.b_st, k.ident_b], writes=[k.psb[6]])
    S.op("dve", lambda e: e.tensor_copy(out=m.mc[:, 0:16], in_=tp[:, 0:16]), reads=[k.psb[6]], writes=[m.b_mc])
    S.op("dve", lambda e: e.tensor_scalar_add(out=m.mc[:, 16:32], in0=tp[:, 16:32], scalar1=1.0),
         reads=[k.psb[6]], writes=[m.b_mc])
    for v in range(2):
        S.dma("sp", m.gb[v][:], k.MODROW[l, v, (s * 3 + 2) * D:(s * 3 + 3) * D].partition_broadcast(128),
              reads=[k.modrow_b[l]], writes=[m.b_gb])
    S.dma("sp", m.lng[:], k.ln_g[l, s, :].partition_broadcast(128), writes=[m.b_ln])
    S.dma("sp", m.lnb[:], k.ln_b[l, s, :].partition_broadcast(128), writes=[m.b_ln])
    m.shift = lambda fc, v: m.mc[:, (0 * 2 + v) * 8 + fc:(0 * 2 + v) * 8 + fc + 1]
    m.scale = lambda fc, v: m.mc[:, (1 * 2 + v) * 8 + fc:(1 * 2 + v) * 8 + fc + 1]
    return m


def transpose_modulate(k, m, xt, b_xt, v, hT, b_hT, col0, tpi, banks=(6, 7)):
    S = k.S
    for half in range(2):
        pb = banks[tpi[0] % len(banks)]
        tpi[0] += 1
        tp = k.ps[pb]
        for q in range(4):
            fc = half * 4 + q
            S.op("pe", lambda e, fc=fc, q=q, tp=tp: e.transpose(out=tp[:, q * 128:(q + 1) * 128],
                                                               in_=xt[:, fc * 128:(fc + 1) * 128], identity=k.ident[:]),
                 reads=[b_xt, k.ident_b], writes=[k.psb[pb]], inc=(q == 3))
        for q in range(4):
            fc = half * 4 + q
            dst = hT[:, fc, col0:col0 + 128]
            src = tp[:, q * 128:(q + 1) * 128]
            if m is None:
                if q % 2 == 0:
                    S.op("act", lambda e, dst=dst, src=src: e.activation(out=dst, in_=src, func=AF.Copy),
                         reads=[k.psb[pb]], writes=[b_hT])
                else:
                    S.op("dve", lambda e, dst=dst, src=src: e.tensor_copy(out=dst, in_=src), reads=[k.psb[pb]], writes=[b_hT])
            elif q % 2 == 0:
                S.op("act", lambda e, fc=fc, dst=dst, src=src: e.activation(out=dst, in_=src, func=AF.Identity,
                                                                           bias=m.shift(fc, v), scale=m.scale(fc, v)),
                     reads=[k.psb[pb], m.b_mc], writes=[b_hT])
            else:
                S.op("dve", lambda e, fc=fc, dst=dst, src=src: e.tensor_scalar(out=dst, in0=src, scalar1=m.scale(fc, v),
                                                                              scalar2=m.shift(fc, v), op0=ALU.mult, op1=ALU.add),
                     reads=[k.psb[pb], m.b_mc], writes=[b_hT])


def pn_part(k, m, v, weight, h, yp, yb, ot, b_ot):
    k.S.op("dve", lambda e: e.scalar_tensor_tensor(out=ot[:, h * 512:(h + 1) * 512], in0=yp, scalar=float(weight),
                                                  in1=m.gb[v][:, h * 512:(h + 1) * 512], op0=ALU.mult, op1=ALU.mult),
           reads=[yb, m.b_gb], writes=[b_ot])


def pn_finish(k, m, xr, b_xr, ot, b_ot, small, b_small, dst, dst_bufs, pool_ok=True):
    S = k.S
    S.op("dve", lambda e: e.scalar_tensor_tensor(out=xr[:], in0=xr[:], scalar=float(ALPHA), in1=ot[:], op0=ALU.mult, op1=ALU.add),
         reads=[b_xr, b_ot], writes=[b_xr])
    layer_norm_rows(k, xr, b_xr, ot, b_ot, small, b_small)
    eng = "pool" if pool_ok else "dve"
    S.op(eng, lambda e: e.tensor_tensor(out=ot[:], in0=ot[:], in1=m.lng[:], op=ALU.mult), reads=[b_ot, m.b_ln], writes=[b_ot])
    S.op(eng, lambda e: e.tensor_tensor(out=ot[:], in0=ot[:], in1=m.lnb[:], op=ALU.add), reads=[b_ot, m.b_ln], writes=[b_ot])
    S.dma("sp", dst, ot[:], reads=[b_ot], writes=dst_bufs)


def post_norm(k, m, v, weight, ypairs, xr, b_xr, ot, b_ot, small, b_small, dst, dst_bufs, pool_ok=True):
    for h, (yp, yb) in enumerate(ypairs):
        pn_part(k, m, v, weight, h, yp, yb, ot, b_ot)
    pn_finish(k, m, xr, b_xr, ot, b_ot, small, b_small, dst, dst_bufs, pool_ok)


def layer_norm_rows(k, src, b_src, ot, b_ot, small, b_small):
    S = k.S
    st = small[:, 0:12]
    mv = small[:, 12:14]
    sd = small[:, 14:15]
    rstd = small[:, 15:16]
    nmr = small[:, 16:17]
    for h in range(2):
        S.op("dve", lambda e, h=h: e.bn_stats(out=small[:, h * 6:(h + 1) * 6], in_=src[:, h * 512:(h + 1) * 512]),
             reads=[b_src], writes=[b_small])
    S.op("dve", lambda e: e.bn_aggr(out=mv, in_=st), reads=[b_small], writes=[b_small])
    S.op("act", lambda e: e.activation(out=sd, in_=small[:, 13:14], func=AF.Sqrt, bias=float(EPS), scale=1.0),
         reads=[b_small], writes=[b_small])
    S.op("dve", lambda e: e.reciprocal(out=rstd, in_=sd), reads=[b_small], writes=[b_small])
    S.op("dve", lambda e: e.tensor_scalar(out=nmr, in0=small[:, 12:13], scalar1=rstd, scalar2=-1.0, op0=ALU.mult, op1=ALU.mult),
         reads=[b_small], writes=[b_small])
    S.op("act", lambda e: e.activation(out=ot[:], in_=src[:], func=AF.Identity, bias=nmr, scale=rstd),
         reads=[b_src, b_small], writes=[b_ot])


def phase_ffn(k, l, half, s, src_fn, ntiles):
    nc, S = k.nc, k.S
    with ExitStack() as es:
        w1 = _sb(k, es, "w1", [128, 8, 2 * DFF], BF16)
        w2 = _sb(k, es, "w2", [128, NJ, D], BF16)
        b_w1, b_w2 = Buf(), Buf()
        w1src = k.ffn_w_in[l, half].rearrange("(c p) n -> p c n", p=128)
        for c0 in range(0, 2 * DFF, 1408):
            for kc in range(8):
                S.dma("pool", w1[:, kc, c0:c0 + 1408], w1src[:, kc, c0:c0 + 1408], writes=[b_w1])
        w2src = k.ffn_w_out[l, half].rearrange("(c p) n -> p c n", p=128)
        for jc in range(NJ):
            S.dma("pool", w2[:, jc, :], w2src[:, jc, :], writes=[b_w2])
        m = load_mod(k, es, l, s, "f")
        hT = _sb(k, es, "hT", [128, 8, 512], BF16)
        aT = _sb(k, es, "aT", [128, NJ, 512], BF16)
        b_hT, b_aT = Buf(), Buf()
        xt = [_sb(k, es, "xt%d" % i, [128, D], F32) for i in range(2)]
        xt2 = [_sb(k, es, "xr%d" % i, [128, D], F32) for i in range(2)]
        ot = [_sb(k, es, "ot%d" % i, [128, D], F32) for i in range(2)]
        sg = [_sb(k, es, "sg%d" % i, [128, 512], BF16) for i in range(2)]
        small = [_sb(k, es, "sm%d" % i, [128, 32], F32) for i in range(2)]
        b_xt, b_xt2, b_ot, b_sg, b_small = ([Buf(), Buf()] for _ in range(5))
        groups = []
        t = 0
        while t < min(ntiles, NTX):
            groups.append((list(range(t, min(t + 4, NTX))), 0))
            t += 4
        if ntiles > NTX:
            groups.append((list(range(NTX, ntiles)), 1))
        tpi = [0]
        xi = 0
        ri = 0
        gi = 0
        for tiles, v in groups:
            n = len(tiles) * 128
            for i, t in enumerate(tiles):
                x_ = xt[xi % 2]
                bx = b_xt[xi % 2]
                xi += 1
                S.dma("sp", x_[:], src_fn(t), reads=[k.Xb[t]], writes=[bx])
                transpose_modulate(k, m, x_, bx, v, hT, b_hT, i * 128, tpi)
            for jc in range(NJ):
                pg = gi % 2
                pu = 2 + gi % 2
                gi += 1
                for kc in range(8):
                    S.op("pe", lambda e, kc=kc, jc=jc, pg=pg: e.matmul(k.ps[pg][:, 0:n], lhsT=w1[:, kc, jc * 128:(jc + 1) * 128],
                                                                      rhs=hT[:, kc, 0:n], start=(kc == 0), stop=(kc == 7)),
                         reads=[b_w1, b_hT], writes=[k.psb[pg]], inc=(kc == 7))
                for kc in range(8):
                    S.op("pe", lambda e, kc=kc, jc=jc, pu=pu: e.matmul(k.ps[pu][:, 0:n], lhsT=w1[:, kc, DFF + jc * 128:DFF + (jc + 1) * 128],
                                                                      rhs=hT[:, kc, 0:n], start=(kc == 0), stop=(kc == 7)),
                         reads=[b_w1, b_hT], writes=[k.psb[pu]], inc=(kc == 7))
                sg_ = sg[jc % 2]
                bs = b_sg[jc % 2]
                S.op("act", lambda e, pg=pg, sg_=sg_: e.activation(out=sg_[:, 0:n], in_=k.ps[pg][:, 0:n], func=AF.Silu),
                     reads=[k.psb[pg]], writes=[bs])
                S.op("dve", lambda e, pu=pu, sg_=sg_, jc=jc: e.tensor_tensor(out=aT[:, jc, 0:n], in0=sg_[:, 0:n], in1=k.ps[pu][:, 0:n],
                                                                           op=ALU.mult),
                     reads=[bs, k.psb[pu]], writes=[b_aT])
            for i, t in enumerate(tiles):
                for h in range(2):
                    for jc in range(NJ):
                        S.op("pe", lambda e, jc=jc, h=h, i=i: e.matmul(k.ps[4 + h][:, :], lhsT=aT[:, jc, i * 128:(i + 1) * 128],
                                                                      rhs=w2[:, jc, h * 512:(h + 1) * 512],
                                                                      start=(jc == 0), stop=(jc == NJ - 1)),
                             reads=[b_aT, b_w2], writes=[k.psb[4 + h]], inc=(jc == NJ - 1))
                r_ = xt2[ri % 2]
                br = b_xt2[ri % 2]
                o_ = ot[ri % 2]
                bo = b_ot[ri % 2]
                sm = small[ri % 2]
                bsm = b_small[ri % 2]
                ri += 1
                S.dma("sp", r_[:], src_fn(t), reads=[k.Xb[t]], writes=[br])
                post_norm(k, m, v, 0.5, [(k.ps[4][:, :], k.psb[4]), (k.ps[5][:, :], k.psb[5])], r_, br, o_, bo, sm, bsm,
                          k.X[t * 128:(t + 1) * 128, :], [k.Xb[t]])
        S.barrier()


def token_groups(ntiles=NT):
    groups = []
    t = 0
    while t < min(ntiles, NTX):
        groups.append((list(range(t, min(t + 4, NTX))), 0))
        t += 4
    if ntiles > NTX:
        groups.append((list(range(NTX, ntiles)), 1))
    return groups


def rope_evac(k, ps_ap, ps_buf, psp_ap, psp_buf, n, dst_ap, b_dst, rc, rs, b_rt, W):
    S = k.S
    S.op("dve", lambda e: e.tensor_tensor(out=W.t1[:, 0:n], in0=ps_ap, in1=rc[:, 0:n], op=ALU.mult),
         reads=[ps_buf, b_rt], writes=[W.b_t1])
    S.op("dve", lambda e: e.tensor_tensor(out=W.t2[:, 0:n], in0=psp_ap, in1=rs[:, 0:n], op=ALU.mult),
         reads=[psp_buf, b_rt], writes=[W.b_t2])
    S.op("dve", lambda e: e.tensor_tensor(out=dst_ap, in0=W.t1[:, 0:n], in1=W.t2[:, 0:n], op=ALU.add),
         reads=[W.b_t1, W.b_t2], writes=[b_dst])


def build_partner_weights(k, wP, b_wP, wX, b_wX, ncols):
    S = k.S
    g = ncols // 32
    for kc in range(8):
        src = wX[:, kc, 0:ncols].rearrange("p (g h f) -> p g h f", g=g, h=2)
        dst = wP[:, kc, 0:ncols].rearrange("p (g h f) -> p g h f", g=g, h=2)
        S.op("act", lambda e, src=src, dst=dst: e.mul(out=dst[:, :, 0, :], in_=src[:, :, 1, :], mul=-1.0), reads=[b_wX], writes=[b_wP])
        S.op("dve", lambda e, src=src, dst=dst: e.tensor_copy(out=dst[:, :, 1, :], in_=src[:, :, 0, :]), reads=[b_wX], writes=[b_wP])


def load_attn_consts(k, es):
    S = k.S
    k.perm = _sb(k, es, "perm", [128, 128], BF16)
    k.maskp = _sb(k, es, "maskp", [128, 512], BF16)
    k.maskn = _sb(k, es, "maskn", [128, 512], BF16)
    k.g08 = _sb(k, es, "g08", [128, 128], F32)
    k.esink = _sb(k, es, "esink", [128, 8], F32)
    k.dl = _sb(k, es, "dl", [128, 4, 64], F32)
    k.lsm = _sb(k, es, "lsm", [128, 2, 64], F32)
    k.lam = _sb(k, es, "lam", [128, 8], F32)
    k.b_cst = Buf()
    S.dma("pool", k.perm[:], k.perm_in[:, :], writes=[k.b_cst])
    S.dma("pool", k.maskp[:], k.maskp_in[:, :], writes=[k.b_cst])
    S.dma("pool", k.maskn[:], k.maskn_in[:, :], writes=[k.b_cst])
    S.dma("sp", k.g08[:], k.diff_norm_g[0, :].partition_broadcast(128), writes=[k.b_cst])
    S.dma("sp", k.esink[:], k.sink_logits[0, :].partition_broadcast(128), writes=[k.b_cst])
    S.dma("sp", k.dl[:].rearrange("p a b -> p (a b)"), k.diff_lambda[0].rearrange("a b -> (a b)").partition_broadcast(128),
          writes=[k.b_cst])
    S.op("dve", lambda e: e.tensor_scalar_mul(out=k.g08[:], in0=k.g08[:], scalar1=0.8), reads=[k.b_cst], writes=[k.b_cst])
    S.op("act", lambda e: e.activation(out=k.esink[:], in_=k.esink[:], func=AF.Exp), reads=[k.b_cst], writes=[k.b_cst])
    S.op("dve", lambda e: e.tensor_tensor(out=k.lsm[:, 0, :], in0=k.dl[:, 0, :], in1=k.dl[:, 1, :], op=ALU.mult),
         reads=[k.b_cst], writes=[k.b_cst])
    S.op("dve", lambda e: e.tensor_tensor(out=k.lsm[:, 1, :], in0=k.dl[:, 2, :], in1=k.dl[:, 3, :], op=ALU.mult),
         reads=[k.b_cst], writes=[k.b_cst])
    S.op("dve", lambda e: e.reduce_sum(out=k.lam[:, 0:2], in_=k.lsm[:], axis=mybir.AxisListType.X), reads=[k.b_cst], writes=[k.b_cst])
    S.op("act", lambda e: e.activation(out=k.lam[:, 2:4], in_=k.lam[:, 0:2], func=AF.Exp), reads=[k.b_cst], writes=[k.b_cst])
    S.op("dve", lambda e: e.tensor_tensor(out=k.lam[:, 4:5], in0=k.lam[:, 3:4], in1=k.lam[:, 2:3], op=ALU.subtract),
         reads=[k.b_cst], writes=[k.b_cst])
    S.op("dve", lambda e: e.tensor_scalar_add(out=k.lam[:, 5:6], in0=k.lam[:, 4:5], scalar1=-0.2), reads=[k.b_cst], writes=[k.b_cst])
    k.neglam = k.lam[:, 5:6]


def attn_inproj(k, es, m, wX, b_wX, wP, b_wP, specs, vspec, tag):
    S = k.S
    W = K()
    W.t1 = _sb(k, es, "t1" + tag, [128, 512], F32)
    W.t2 = _sb(k, es, "t2" + tag, [128, 512], F32)
    W.b_t1, W.b_t2 = Buf(), Buf()
    hT = _sb(k, es, "hT" + tag, [128, 8, 512], BF16)
    b_hT = Buf()
    xt = [_sb(k, es, "xt%d" % i + tag, [128, D], F32) for i in range(2)]
    b_xt = [Buf(), Buf()]
    rc = [_sb(k, es, "rc%d" % i + tag, [128, 512], F32) for i in range(2)]
    rs = [_sb(k, es, "rs%d" % i + tag, [128, 512], F32) for i in range(2)]
    b_rt = [Buf(), Buf()]
    tpi = [0]
    xi = pj = pp = vv = 0
    for gidx, (tiles, v) in enumerate(token_groups()):
        n = len(tiles) * 128
        tok0 = tiles[0] * 128
        for i, t in enumerate(tiles):
            x_ = xt[xi % 2]
            bx = b_xt[xi % 2]
            xi += 1
            S.dma("sp", x_[:], k.X[t * 128:(t + 1) * 128, :], reads=[k.Xb[t]], writes=[bx])
            transpose_modulate(k, m, x_, bx, v, hT, b_hT, i * 128, tpi)
        if v == 0:
            rc_, rs_, brt = rc[gidx % 2], rs[gidx % 2], b_rt[gidx % 2]
            S.dma("sp", rc_[:, 0:n], k.ropec_in[:, tok0:tok0 + n], writes=[brt])
            S.dma("sp", rs_[:, 0:n], k.ropes_in[:, tok0:tok0 + n], writes=[brt])
        for dst, b_dst, col0, nch in specs:
            for h in range(nch):
                pb = pj % 2
                pj += 1
                for kc in range(8):
                    S.op("pe", lambda e, kc=kc, h=h, pb=pb, col0=col0: e.matmul(k.ps[pb][:, 0:n], lhsT=wX[:, kc, col0 + h * 128:col0 + (h + 1) * 128],
                                                                               rhs=hT[:, kc, 0:n], start=(kc == 0), stop=(kc == 7)),
                         reads=[b_wX, b_hT], writes=[k.psb[pb]], inc=(kc == 7))
                d_ap = dst[:, h, tok0:tok0 + n]
                if v == 0:
                    pq = 2 + pp % 2
                    pp += 1
                    for kc in range(8):
                        S.op("pe", lambda e, kc=kc, h=h, pq=pq, col0=col0: e.matmul(k.ps[pq][:, 0:n], lhsT=wP[:, kc, col0 + h * 128:col0 + (h + 1) * 128],
                                                                                   rhs=hT[:, kc, 0:n], start=(kc == 0), stop=(kc == 7)),
                             reads=[b_wP, b_hT], writes=[k.psb[pq]], inc=(kc == 7))
                    rope_evac(k, k.ps[pb][:, 0:n], k.psb[pb], k.ps[pq][:, 0:n], k.psb[pq], n, d_ap, b_dst, rc_, rs_, brt, W)
                else:
                    S.op("act", lambda e, d_ap=d_ap, pb=pb: e.activation(out=d_ap, in_=k.ps[pb][:, 0:n], func=AF.Copy),
                         reads=[k.psb[pb]], writes=[b_dst])
        vdst, b_vdst, vcol0, nh, dv = vspec
        for i, t in enumerate(tiles if not os.environ.get("ATT_NOV") else []):
            pb = 4 + vv % 2
            vv += 1
            for kc in range(8):
                S.op("pe", lambda e, kc=kc, i=i, pb=pb: e.matmul(k.ps[pb][:, 0:nh * dv], lhsT=hT[:, kc, i * 128:(i + 1) * 128],
                                                                rhs=wX[:, kc, vcol0:vcol0 + nh * dv], start=(kc == 0), stop=(kc == 7)),
                     reads=[b_wX, b_hT], writes=[k.psb[pb]], inc=(kc == 7))
            S.op("dve", lambda e, t=t, pb=pb: e.tensor_copy(out=vdst[:, t, :, 0:dv],
                                                           in_=k.ps[pb][:, 0:nh * dv].rearrange("p (h d) -> p h d", h=nh)),
                 reads=[k.psb[pb]], writes=[b_vdst])


def phase_attn_a(k):
    nc, S = k.nc, k.S
    with ExitStack() as es:
        wA = _sb(k, es, "wA", [128, 8, 1536], BF16)
        b_wA = Buf()
        wsrc = k.attn_w_in[0].rearrange("(c p) n -> p c n", p=128)
        for kc in range(8):
            S.dma("pool", wA[:, kc, :], wsrc[:, kc, 0:1536], writes=[b_wA])
        m = load_mod(k, es, 0, 1, "a")
        QT = _sb(k, es, "QTA", [128, 4, NT * 128], BF16)
        KT = _sb(k, es, "KTA", [128, 4, NT * 128], BF16)
        VA = _sb(k, es, "VA", [128, NT, 4, 132], BF16)
        b_QT, b_KT, b_VA = Buf(), Buf(), Buf()
        S.op("pool", lambda e: e.memset(VA[:].rearrange("p a b c -> p (a b c)"), 1.0), writes=[b_VA])
        if True:
            wPA = _sb(k, es, "wPA", [128, 8, 1024], BF16)
            b_wPA = Buf()
            build_partner_weights(k, wPA, b_wPA, wA, b_wA, 1024)
            attn_inproj(k, es, m, wA, b_wA, wPA, b_wPA, [(QT, b_QT, 0, 4), (KT, b_KT, 512, 4)], (VA, b_VA, 1024, 4, 128), "a")
        if k.stop_after == "attn_ai":
            S.barrier()
            return
        PT = [_sb(k, es, "PT%d" % i, [128, 512], BF16) for i in range(2)]
        b_PT = [Buf(), Buf()]
        AB = [_sb(k, es, "ABa%d" % i, [128, 512], F32) for i in range(4)]
        b_AB = [Buf() for _ in range(4)]
        ta = [_sb(k, es, "ta%d" % i, [128, 128], F32) for i in range(2)]
        junk = [_sb(k, es, "junk%d" % i, [128, 128], F32) for i in range(2)]
        sm = [_sb(k, es, "asm%d" % i, [128, 8], F32) for i in range(2)]
        b_ta, b_junk, b_sm = [Buf(), Buf()], [Buf(), Buf()], [Buf(), Buf()]
        qgroups = [(g * 256, list(range(NT))) for g in range(T // 256)] + [(T, [NTX, NTX + 1])]
        si = fi = 0
        for qg, (q0, ktiles) in enumerate(qgroups):
            for h in range(4):
                for ki, kt in enumerate(ktiles):
                    pbs = (0, 1) if si % 2 == 0 else (6, 7)
                    PT_ = PT[si % 2]
                    bpt = b_PT[si % 2]
                    si += 1
                    for sub in range(2):
                        S.op("pe", lambda e, sub=sub, h=h, kt=kt, pbs=pbs: e.matmul(
                            k.ps[pbs[sub]][:, 0:256], lhsT=KT[sub * 64:(sub + 1) * 64, h, kt * 128:(kt + 1) * 128],
                            rhs=QT[sub * 64:(sub + 1) * 64, h, q0:q0 + 256], start=True, stop=True),
                             reads=[b_KT, b_QT], writes=[k.psb[pbs[sub]]])
                    for sub in range(2):
                        S.op("act", lambda e, sub=sub, pbs=pbs, PT_=PT_: e.activation(out=PT_[:, sub * 256:(sub + 1) * 256], in_=k.ps[pbs[sub]][:, 0:256],
                                                                                  func=AF.Exp, scale=0.125),
                             reads=[k.psb[pbs[sub]]], writes=[bpt])
                    last = ki == len(ktiles) - 1
                    for sub in range(2):
                        for qt in range(2):
                            ab = 2 + sub * 2 + qt
                            S.op("pe", lambda e, sub=sub, qt=qt, ab=ab, PT_=PT_, kt=kt, h=h, ki=ki, last=last: e.matmul(
                                k.ps[ab][:, 0:129], lhsT=PT_[:, sub * 256 + qt * 128:sub * 256 + (qt + 1) * 128],
                                rhs=VA[:, kt, h, 0:129], start=(ki == 0), stop=last),
                                 reads=[bpt, b_VA], writes=[k.psb[ab]], inc=(last and sub == 1 and qt == 1))
                for qt in range(2):
                    b1, b2 = 2 + qt, 4 + qt
                    s_ = sm[fi % 2]
                    bs = b_sm[fi % 2]
                    t_ = ta[fi % 2]
                    bt = b_ta[fi % 2]
                    j_ = junk[fi % 2]
                    bj = b_junk[fi % 2]
                    fi += 1
                    ab_ = AB[(qg % 2) * 2 + qt]
                    bab = b_AB[(qg % 2) * 2 + qt]
                    S.op("dve", lambda e, s_=s_, b1=b1: e.reciprocal(out=s_[:, 0:1], in_=k.ps[b1][:, 128:129]), reads=[k.psb[b1]], writes=[bs])
                    S.op("dve", lambda e, s_=s_, b2=b2: e.reciprocal(out=s_[:, 1:2], in_=k.ps[b2][:, 128:129]), reads=[k.psb[b2]], writes=[bs])
                    S.op("dve", lambda e, s_=s_: e.tensor_scalar(out=s_[:, 2:3], in0=s_[:, 1:2], scalar1=k.neglam, scalar2=None, op0=ALU.mult),
                         reads=[bs, k.b_cst], writes=[bs])
                    S.op("dve", lambda e, s_=s_, t_=t_, b1=b1: e.tensor_scalar(out=t_[:], in0=k.ps[b1][:, 0:128], scalar1=s_[:, 0:1], scalar2=None,
                                                                              op0=ALU.mult), reads=[k.psb[b1], bs], writes=[bt])
                    S.op("dve", lambda e, s_=s_, t_=t_, b2=b2: e.scalar_tensor_tensor(out=t_[:], in0=k.ps[b2][:, 0:128], scalar=s_[:, 2:3], in1=t_[:],
                                                                                     op0=ALU.mult, op1=ALU.add),
                         reads=[k.psb[b2], bs, bt], writes=[bt])
                    S.op("act", lambda e, s_=s_, t_=t_, j_=j_: e.activation(out=j_[:], in_=t_[:], func=AF.Square, accum_out=s_[:, 3:4]),
                         reads=[bt], writes=[bj, bs])
                    S.op("act", lambda e, s_=s_: e.activation(out=s_[:, 4:5], in_=s_[:, 3:4], func=AF.Sqrt, scale=1.0 / 128.0, bias=float(EPS)),
                         reads=[bs], writes=[bs])
                    S.op("dve", lambda e, s_=s_: e.reciprocal(out=s_[:, 5:6], in_=s_[:, 4:5]), reads=[bs], writes=[bs])
                    S.op("dve", lambda e, s_=s_, t_=t_, ab_=ab_, h=h: e.scalar_tensor_tensor(out=ab_[:, h * 128:(h + 1) * 128], in0=t_[:], scalar=s_[:, 5:6],
                                                                                           in1=k.g08[:], op0=ALU.mult, op1=ALU.mult),
                         reads=[bt, bs, k.b_cst], writes=[bab])
            for qt in range(2):
                t = q0 // 128 + qt
                S.dma("sp", k.ABd[t * 128:(t + 1) * 128, :], AB[(qg % 2) * 2 + qt][:], reads=[b_AB[(qg % 2) * 2 + qt]], writes=[k.ABb[t]])
        S.barrier()


def phase_attn_b(k):
    nc, S = k.nc, k.S
    with ExitStack() as es:
        wB = _sb(k, es, "wB", [128, 8, 768], BF16)
        wo = _sb(k, es, "wo", [128, 8, D], BF16)
        b_wB, b_wo = Buf(), Buf()
        wsrc = k.attn_w_in[0].rearrange("(c p) n -> p c n", p=128)
        for kc in range(8):
            for hf in range(2):
                S.dma("pool", wB[:, kc, 0:512].rearrange("p (g hf d) -> p g hf d", g=4, hf=2)[:, :, hf, :],
                      wsrc[:, kc, 1536:2048].rearrange("p (hf g d) -> p hf g d", g=4, hf=2)[:, hf, :, :], writes=[b_wB])
            S.dma("pool", wB[:, kc, 512:768], wsrc[:, kc, 2048:2304], writes=[b_wB])
        wosrc = k.attn_w_out[0].rearrange("(c p) n -> p c n", p=128)
        for kc in range(8):
            S.dma("pool", wo[:, kc, :], wosrc[:, kc, :], writes=[b_wo])
        m = load_mod(k, es, 0, 1, "b")
        QT = _sb(k, es, "QTB", [128, 4, NT * 128], BF16)
        KT = _sb(k, es, "KTB", [128, 1, NT * 128], BF16)
        VB = _sb(k, es, "VB", [128, NT, 2, 66], BF16)
        b_QT, b_KT, b_VB = Buf(), Buf(), Buf()
        S.op("pool", lambda e: e.memset(VB[:].rearrange("p a b c -> p (a b c)"), 1.0), writes=[b_VB])
        if True:
            wPB = _sb(k, es, "wPB", [128, 8, 640], BF16)
            b_wPB = Buf()
            build_partner_weights(k, wPB, b_wPB, wB, b_wB, 640)
            attn_inproj(k, es, m, wB, b_wB, wPB, b_wPB, [(QT, b_QT, 0, 4), (KT, b_KT, 512, 1)], (VB, b_VB, 640, 2, 64), "b")
        PT = [_sb(k, es, "PTb%d" % i, [128, 512], BF16) for i in range(2)]
        b_PT = [Buf(), Buf()]
        AB = [_sb(k, es, "ABb%d" % i, [128, D], F32) for i in range(2)]
        b_AB = [Buf(), Buf()]
        abT = _sb(k, es, "abT", [128, 8, 128], BF16)
        b_abT = Buf()
        xr = [_sb(k, es, "xrb%d" % i, [128, D], F32) for i in range(2)]
        ot = [_sb(k, es, "otb%d" % i, [128, D], F32) for i in range(2)]
        small = [_sb(k, es, "smb%d" % i, [128, 32], F32) for i in range(2)]
        sm = [_sb(k, es, "bsm%d" % i, [128, 8], F32) for i in range(2)]
        b_xr, b_ot, b_small, b_sm = ([Buf(), Buf()] for _ in range(4))
        si = fi = 0
        tpi = [0]
        for qt in range(NT):
            v = 0 if qt < NTX else 1
            if v == 0:
                kts = ([(qt - 1, k.maskp)] if qt > 0 else []) + [(qt, None)] + ([(qt + 1, k.maskn)] if qt < NTX - 1 else [])
                kts += [(NTX, None), (NTX + 1, None)]
            else:
                kts = [(NTX, None), (NTX + 1, None)]
            abt = AB[qt % 2]
            bab = b_AB[qt % 2]
            S.dma("sp", abt[:, 0:512], k.ABd[qt * 128:(qt + 1) * 128, :], reads=[k.ABb[qt]], writes=[bab])
            for kvh in range(2):
                p0, p1 = kvh * 64, (kvh + 1) * 64
                for ki, (kt, mk) in enumerate(kts):
                    pb = si % 2
                    PT_ = PT[si % 2]
                    bpt = b_PT[si % 2]
                    si += 1
                    S.op("pe", lambda e, kt=kt, pb=pb, p0=p0, p1=p1: e.matmul(
                        k.ps[pb][:, :].rearrange("p (g q) -> p g q", g=4), lhsT=KT[p0:p1, 0, kt * 128:(kt + 1) * 128],
                        rhs=QT[p0:p1, :, qt * 128:(qt + 1) * 128], start=True, stop=True),
                         reads=[b_KT, b_QT], writes=[k.psb[pb]])
                    S.op("act", lambda e, pb=pb, PT_=PT_: e.activation(out=PT_[:], in_=k.ps[pb][:, :], func=AF.Exp, scale=0.125),
                         reads=[k.psb[pb]], writes=[bpt])
                    if mk is not None:
                        S.op("pool", lambda e, PT_=PT_, mk=mk: e.tensor_tensor(out=PT_[:], in0=PT_[:], in1=mk[:], op=ALU.mult),
                             reads=[bpt, k.b_cst], writes=[bpt])
                    last = ki == len(kts) - 1
                    for g in range(4):
                        S.op("pe", lambda e, g=g, PT_=PT_, kt=kt, kvh=kvh, ki=ki, last=last: e.matmul(
                            k.ps[2 + g][:, 0:65], lhsT=PT_[:, g * 128:(g + 1) * 128], rhs=VB[:, kt, kvh, 0:65], start=(ki == 0), stop=last),
                             reads=[bpt, b_VB], writes=[k.psb[2 + g]], inc=(last and g == 3))
                for g in range(4):
                    head = kvh * 4 + g
                    s_ = sm[fi % 2]
                    bs = b_sm[fi % 2]
                    fi += 1
                    S.op("dve", lambda e, s_=s_, g=g, head=head: e.tensor_tensor(out=s_[:, 0:1], in0=k.ps[2 + g][:, 64:65],
                                                                                in1=k.esink[:, head:head + 1], op=ALU.add),
                         reads=[k.psb[2 + g], k.b_cst], writes=[bs])
                    S.op("dve", lambda e, s_=s_: e.reciprocal(out=s_[:, 1:2], in_=s_[:, 0:1]), reads=[bs], writes=[bs])
                    S.op("dve", lambda e, s_=s_, g=g, head=head, abt=abt: e.tensor_scalar(out=abt[:, 512 + head * 64:512 + (head + 1) * 64],
                                                                                        in0=k.ps[2 + g][:, 0:64], scalar1=s_[:, 1:2], scalar2=None,
                                                                                        op0=ALU.mult),
                         reads=[k.psb[2 + g], bs], writes=[bab])
            if k.dbg_on:
                S.dma("sp", k.BBd[qt * 128:(qt + 1) * 128, :], abt[:, 512:1024], reads=[bab], writes=[k.BBb[qt]])
            transpose_modulate(k, None, abt, bab, v, abT, b_abT, 0, tpi, banks=(6,))
            xr_, bxr = xr[qt % 2], b_xr[qt % 2]
            ot_, bot = ot[qt % 2], b_ot[qt % 2]
            S.dma("sp", xr_[:], k.X[qt * 128:(qt + 1) * 128, :], reads=[k.Xb[qt]], writes=[bxr])
            for h2 in range(2):
                for fc in range(8):
                    S.op("pe", lambda e, fc=fc, h2=h2: e.matmul(k.ps[7][:, :], lhsT=abT[:, fc, :], rhs=wo[:, fc, h2 * 512:(h2 + 1) * 512],
                                                               start=(fc == 0), stop=(fc == 7)),
                         reads=[b_abT, b_wo], writes=[k.psb[7]], inc=(fc == 7))
                pn_part(k, m, v, 1.0, h2, k.ps[7][:, :], k.psb[7], ot_, bot)
            pn_finish(k, m, xr_, bxr, ot_, bot, small[qt % 2], b_small[qt % 2], k.X[qt * 128:(qt + 1) * 128, :], [k.Xb[qt]])
        S.barrier()


RC = dict(mq=0, mk=512, mv=1024, mo=1536, mg=2048, rq=2064, rk=2320, rv=2576, rg=3088)


def load_rec_consts(k, es):
    S = k.S
    c = K()
    k.rc = c
    c.b = Buf()
    f32 = lambda name, shape: _sb(k, es, name, shape, F32)
    c.tri = [f32("tri%d" % d, [128, 128]) for d in range(2)]
    c.mb = [f32("mb%d" % d, [128, 128]) for d in range(2)]
    c.mw = [f32("mw%d" % d, [128, 128]) for d in range(2)]
    c.mall = f32("mall", [128, 128])
    c.pos = f32("posc", [128, 8])
    c.gb = f32("gateb", [128, 16])
    c.lg = f32("lgam", [128, 8])
    c.rcol = f32("rcol", [128, 32])
    c.mm = [f32("mm%d" % d, [128, 128]) for d in range(2)]
    c.dj = [[f32("dj%d%d" % (d, h), [128, 128]) for h in range(4)] for d in range(2)]
    c.gg = f32("ggrow", [128, D])
    c.rb = f32("rbrow", [128, 512])
    for d in range(2):
        S.dma("sp", c.tri[d][:], k.tri_in[d], writes=[c.b])
        S.dma("sp", c.mb[d][:], k.mb_in[d], writes=[c.b])
        S.dma("sp", c.mw[d][:], k.mw_in[d], writes=[c.b])
    S.dma("sp", c.mall[:], k.mall_in[:, :], writes=[c.b])
    S.dma("sp", c.pos[:], k.pos_in[:, :], writes=[c.b])
    S.dma("sp", c.gb[:], k.mlstm_gate_b[0].rearrange("a b c -> (a b c)").partition_broadcast(128), writes=[c.b])
    S.dma("sp", c.lg[:], k.ret_decay_logit[0].rearrange("a b -> (a b)").partition_broadcast(128), writes=[c.b])
    S.dma("sp", c.gg[:, 0:512], k.mlstm_norm_g[0, :].partition_broadcast(128), writes=[c.b])
    S.dma("sp", c.gg[:, 512:1024], k.ret_norm_g[0, :].partition_broadcast(128), writes=[c.b])
    S.dma("sp", c.rb[:], k.ret_norm_b[0, :].partition_broadcast(128), writes=[c.b])
    rw = [c.b]
    S.op("act", lambda e: e.activation(out=c.lg[:], in_=c.lg[:], func=AF.Exp, scale=-1.0), reads=rw, writes=rw)
    S.op("act", lambda e: e.activation(out=c.lg[:], in_=c.lg[:], func=AF.Ln, bias=1.0, scale=1.0), reads=rw, writes=rw)
    S.op("dve", lambda e: e.tensor_scalar_mul(out=c.lg[:], in0=c.lg[:], scalar1=-1.0), reads=rw, writes=rw)
    for d in range(2):
        S.op("act", lambda e, d=d: e.activation(out=c.rcol[:, d * 4:d * 4 + 4], in_=c.lg[:, d * 4:d * 4 + 4], func=AF.Exp,
                                                scale=c.pos[:, d:d + 1]), reads=rw, writes=rw)
        S.op("act", lambda e, d=d: e.activation(out=c.rcol[:, 8 + d * 4:8 + d * 4 + 4], in_=c.lg[:, d * 4:d * 4 + 4], func=AF.Exp,
                                                scale=c.pos[:, 2 + d:3 + d]), reads=rw, writes=rw)
        S.op("act", lambda e, d=d: e.activation(out=c.rcol[:, 16 + d * 4:16 + d * 4 + 4], in_=c.lg[:, d * 4:d * 4 + 4], func=AF.Exp,
                                                scale=c.pos[:, 4 + d:5 + d]), reads=rw, writes=rw)
    S.op("act", lambda e: e.activation(out=c.rcol[:, 24:32], in_=c.lg[:], func=AF.Exp, scale=128.0), reads=rw, writes=rw)
    for d in range(2):
        S.op("dve", lambda e, d=d: e.tensor_scalar_mul(out=c.mm[d][:], in0=c.tri[d][:], scalar1=float(128.0 ** -0.5)), reads=rw, writes=rw)
        for h in range(4):
            S.op("dve", lambda e, d=d, h=h: e.tensor_scalar(out=c.dj[d][h][:], in0=c.tri[d][:], scalar1=c.rcol[:, d * 4 + h:d * 4 + h + 1],
                                                           scalar2=0.125, op0=ALU.mult, op1=ALU.mult), reads=rw, writes=rw)
    c.glp = f32("glp", [128, 4])
    for d in range(2):
        for p in range(2):
            for hf in range(2):
                r0 = hf * 64
                src = 24 + d * 4 + 2 * p + hf
                S.op("dve", lambda e, d=d, p=p, r0=r0, src=src: e.tensor_copy(out=c.glp[r0:r0 + 64, d * 2 + p:d * 2 + p + 1],
                                                                             in_=c.rcol[r0:r0 + 64, src:src + 1]), reads=rw, writes=rw)


def phase_rec(k, d):
    nc, S = k.nc, k.S
    c = k.rc
    l = 1
    with ExitStack() as es:
        wR = _sb(k, es, "wR", [128, 8, REC_IN], BF16)
        b_wR = Buf()
        wsrc = k.rec_w_in[0].rearrange("(c p) n -> p c n", p=128)
        for kc in range(8):
            for c0 in (0, 1800):
                S.dma("pool", wR[:, kc, c0:c0 + 1800], wsrc[:, kc, c0:c0 + 1800], writes=[b_wR])
        if d == 1:
            wo = _sb(k, es, "wor", [128, 8, D], BF16)
            b_wo = Buf()
            wosrc = k.rec_w_out[0].rearrange("(c p) n -> p c n", p=128)
            for kc in range(8):
                S.dma("pool", wo[:, kc, :], wosrc[:, kc, :], writes=[b_wo])
        m = load_mod(k, es, l, 1, "r%d" % d)
        bf = lambda name, shape: _sb(k, es, name + "_%d" % d, shape, BF16)
        f32 = lambda name, shape: _sb(k, es, name + "_%d" % d, shape, F32)
        xt = [f32("rxt%d" % i, [128, D]) for i in range(2)]
        b_xt = [Buf(), Buf()]
        hT = bf("rhT", [128, 8, 128])
        b_hT = Buf()
        FT = bf("FT", [128, 12, 128])
        b_FT = Buf()
        VA = bf("rVA", [128, 4, 132])
        RV = bf("rRV", [128, 4, 128])
        b_VA, b_RV = Buf(), Buf()
        S.op("pool", lambda e: e.memset(VA[:].rearrange("p a b -> p (a b)"), 1.0), writes=[b_VA])
        G = f32("rG", [128, 8])
        LF = f32("rLF", [128, 4])
        PK = f32("rPK", [128, 16])
        E = f32("rE", [128, 16])
        b_G, b_LF, b_PK, b_E = Buf(), Buf(), Buf(), Buf()
        Sb = [bf("rSb%d" % i, [128, 128]) for i in range(2)]
        b_Sb = [Buf(), Buf()]
        Kpp = [bf("rKpp%d" % i, [128, 128]) for i in range(2)]
        b_Kpp = [Buf(), Buf()]
        Kpad = [[bf("rKpad%d%d" % (p, hf), [128, 128]) for hf in range(2)] for p in range(2)]
        b_Kpad = [[Buf(), Buf()], [Buf(), Buf()]]
        for p in range(2):
            for hf in range(2):
                S.op("pool", lambda e, p=p, hf=hf: e.memset(Kpad[p][hf][:], 0.0), writes=[b_Kpad[p][hf]])
        C = [f32("rC%d" % h, [128, 132]) for h in range(4)]
        Cb = [bf("rCb%d" % h, [128, 132]) for h in range(4)]
        b_C, b_Cb = [Buf() for _ in range(4)], [Buf() for _ in range(4)]
        Sp = [f32("rSp%d" % p, [128, 128]) for p in range(2)]
        Spb = [bf("rSpb%d" % p, [128, 128]) for p in range(2)]
        b_Sp, b_Spb = [Buf(), Buf()], [Buf(), Buf()]
        for h in range(4):
            S.op("pool", lambda e, h=h: e.memset(C[h][:], 0.0), writes=[b_C[h]])
            S.op("pool", lambda e, h=h: e.memset(Cb[h][:], 0.0), writes=[b_Cb[h]])
        for p in range(2):
            S.op("pool", lambda e, p=p: e.memset(Sp[p][:], 0.0), writes=[b_Sp[p]])
            S.op("pool", lambda e, p=p: e.memset(Spb[p][:], 0.0), writes=[b_Spb[p]])
        H = [f32("rH%d" % i, [128, D]) for i in range(2)]
        b_H = [Buf(), Buf()]
        fs = [f32("rfs%d" % i, [128, 8]) for i in range(2)]
        b_fs = [Buf(), Buf()]
        if d == 1:
            H0 = [f32("rH0%d" % i, [128, D]) for i in range(2)]
            b_H0 = [Buf(), Buf()]
            gsig = f32("rgs", [128, D])
            b_gsig = Buf()
            stt = f32("rstt", [128, 8, 6])
            mv8 = f32("rmv8", [128, 8, 2])
            sc8 = f32("rsc8", [128, 24])
            b_st = Buf()
            zT = bf("rzT", [128, 8, 128])
            b_zT = Buf()
            xr = [f32("rxr%d" % i, [128, D]) for i in range(2)]
            ot = [f32("rot%d" % i, [128, D]) for i in range(2)]
            small = [f32("rsm%d" % i, [128, 32]) for i in range(2)]
            b_xr, b_ot, b_small = ([Buf(), Buf()] for _ in range(3))
        order = [NTX, NTX + 1] + list(range(NTX)) if d == 0 else [NTX + 1, NTX] + list(range(NTX - 1, -1, -1))
        tpi = [0]
        si = ki = fi = 0
        for ci, t in enumerate(order):
            v = 0 if t < NTX else 1
            need_out = v == 0
            x_, bx = xt[ci % 2], b_xt[ci % 2]
            S.dma("sp", x_[:], k.X[t * 128:(t + 1) * 128, :], reads=[k.Xb[t]], writes=[bx])
            transpose_modulate(k, m, x_, bx, v, hT, b_hT, 0, tpi, banks=(6,))
            fcols = [RC["mq"] + h * 128 for h in range(4)] + [RC["mk"] + h * 128 for h in range(4)] + \
                    [RC["rq"], RC["rq"] + 128, RC["rk"], RC["rk"] + 128]
            for grp in range(3):
                pb = grp % 2
                for j4 in range(4):
                    col = fcols[grp * 4 + j4]
                    for kc in range(8):
                        S.op("pe", lambda e, kc=kc, col=col, j4=j4, pb=pb: e.matmul(k.ps[pb][:, j4 * 128:(j4 + 1) * 128], lhsT=wR[:, kc, col:col + 128],
                                                                                  rhs=hT[:, kc, :], start=(kc == 0), stop=(kc == 7)),
                             reads=[b_wR, b_hT], writes=[k.psb[pb]], inc=(kc == 7 and j4 == 3))
                S.op("act", lambda e, grp=grp, pb=pb: e.activation(out=FT[:, grp * 4:(grp + 1) * 4, :].rearrange("p a b -> p (a b)"), in_=k.ps[pb][:, :],
                                                                  func=AF.Copy), reads=[k.psb[pb]], writes=[b_FT])
            def tm(pb, col, n, c0=0, last=True):
                for kc in range(8):
                    S.op("pe", lambda e, kc=kc: e.matmul(k.ps[pb][:, c0:c0 + n], lhsT=hT[:, kc, :], rhs=wR[:, kc, col:col + n],
                                                         start=(kc == 0), stop=(kc == 7)),
                         reads=[b_wR, b_hT], writes=[k.psb[pb]], inc=(kc == 7))
            tm(2, RC["mk"], 512)
            tm(3, RC["mv"], 512)
            tm(4, RC["rk"], 256)
            tm(4, RC["mg"], 16, c0=256)
            tm(5, RC["rv"], 512)
            S.op("dve", lambda e: e.tensor_copy(out=VA[:, :, 0:128], in_=k.ps[3][:, :].rearrange("p (h d) -> p h d", h=4)),
                 reads=[k.psb[3]], writes=[b_VA])
            S.op("act", lambda e: e.activation(out=RV[:].rearrange("p a b -> p (a b)"), in_=k.ps[5][:, :], func=AF.Copy),
                 reads=[k.psb[5]], writes=[b_RV])
            S.op("dve", lambda e: e.tensor_tensor(out=G[:], in0=k.ps[4][:, 256 + d * 8:256 + d * 8 + 8], in1=c.gb[:, d * 8:d * 8 + 8], op=ALU.add),
                 reads=[k.psb[4], c.b], writes=[b_G])
            S.op("act", lambda e: e.activation(out=LF[:], in_=G[:, 4:8], func=AF.Exp, scale=-1.0), reads=[b_G], writes=[b_LF])
            S.op("act", lambda e: e.activation(out=LF[:], in_=LF[:], func=AF.Ln, bias=1.0, scale=1.0), reads=[b_LF], writes=[b_LF])
            for ji, mat in enumerate((c.mb[d], c.mw[d], c.mall)):
                S.op("pe", lambda e, ji=ji, mat=mat: e.matmul(k.ps[7][:, ji * 4:ji * 4 + 4], lhsT=mat[:], rhs=LF[:], start=True, stop=True),
                     reads=[c.b, b_LF], writes=[k.psb[7]], inc=(ji == 2))
            S.op("dve", lambda e: e.tensor_tensor(out=PK[:, 0:4], in0=G[:, 0:4], in1=k.ps[7][:, 0:4], op=ALU.subtract),
                 reads=[b_G, k.psb[7]], writes=[b_PK])
            S.op("dve", lambda e: e.tensor_tensor(out=PK[:, 4:8], in0=G[:, 0:4], in1=k.ps[7][:, 4:8], op=ALU.add),
                 reads=[b_G, k.psb[7]], writes=[b_PK])
            S.op("dve", lambda e: e.tensor_copy(out=PK[:, 8:12], in_=k.ps[7][:, 0:4]), reads=[k.psb[7]], writes=[b_PK])
            S.op("dve", lambda e: e.tensor_copy(out=PK[:, 12:16], in_=k.ps[7][:, 8:12]), reads=[k.psb[7]], writes=[b_PK])
            S.op("act", lambda e: e.activation(out=E[:], in_=PK[:], func=AF.Exp), reads=[b_PK], writes=[b_E])
            H_, bH = H[ci % 2], b_H[ci % 2]
            for h in range(4):
                if need_out:
                    sb_, bsb = Sb[si % 2], b_Sb[si % 2]
                    si += 1
                    S.op("pe", lambda e, h=h: e.matmul(k.ps[0][:, 0:128], lhsT=FT[:, 4 + h, :], rhs=FT[:, h, :], start=True, stop=True),
                         reads=[b_FT], writes=[k.psb[0]])
                    S.op("dve", lambda e, h=h, sb_=sb_: e.scalar_tensor_tensor(out=sb_[:], in0=k.ps[0][:, 0:128], scalar=E[:, h:h + 1], in1=c.mm[d][:],
                                                                              op0=ALU.mult, op1=ALU.mult),
                         reads=[k.psb[0], b_E, c.b], writes=[bsb])
                    S.op("pe", lambda e, h=h, sb_=sb_: e.matmul(k.ps[1][:, 0:129], lhsT=sb_[:], rhs=VA[:, h, 0:129], start=True, stop=False),
                         reads=[bsb, b_VA], writes=[k.psb[1]], inc=False)
                    S.op("pe", lambda e, h=h: e.matmul(k.ps[1][:, 0:129], lhsT=FT[:, h, :], rhs=Cb[h][:, 0:129], start=False, stop=True),
                         reads=[b_FT, b_Cb[h]], writes=[k.psb[1]])
                    f_, bf_ = fs[fi % 2], b_fs[fi % 2]
                    fi += 1
                    S.op("dve", lambda e, h=h, f_=f_: e.tensor_tensor(out=f_[:, 0:1], in0=k.ps[1][:, 128:129], in1=E[:, 8 + h:9 + h], op=ALU.mult),
                         reads=[k.psb[1], b_E], writes=[bf_])
                    S.op("dve", lambda e, f_=f_: e.tensor_scalar_mul(out=f_[:, 4:5], in0=f_[:, 0:1], scalar1=-1.0), reads=[bf_], writes=[bf_])
                    S.op("dve", lambda e, f_=f_: e.tensor_tensor(out=f_[:, 1:2], in0=f_[:, 0:1], in1=f_[:, 4:5], op=ALU.max), reads=[bf_], writes=[bf_])
                    S.op("dve", lambda e, f_=f_: e.tensor_scalar_max(out=f_[:, 1:2], in0=f_[:, 1:2], scalar1=1.0), reads=[bf_], writes=[bf_])
                    S.op("dve", lambda e, f_=f_: e.reciprocal(out=f_[:, 2:3], in_=f_[:, 1:2]), reads=[bf_], writes=[bf_])
                    S.op("dve", lambda e, h=h, f_=f_: e.tensor_tensor(out=f_[:, 3:4], in0=f_[:, 2:3], in1=E[:, 8 + h:9 + h], op=ALU.mult),
                         reads=[bf_, b_E], writes=[bf_])
                    S.op("dve", lambda e, h=h, f_=f_, H_=H_: e.tensor_scalar(out=H_[:, h * 128:(h + 1) * 128], in0=k.ps[1][:, 0:128], scalar1=f_[:, 3:4],
                                                                           scalar2=None, op0=ALU.mult),
                         reads=[k.psb[1], bf_], writes=[bH])
                kp_, bkp = Kpp[ki % 2], b_Kpp[ki % 2]
                ki += 1
                S.op("act" if h % 2 == 0 else "dve",
                     (lambda e, h=h, kp_=kp_: e.activation(out=kp_[:], in_=k.ps[2][:, h * 128:(h + 1) * 128], func=AF.Identity, scale=E[:, 4 + h:5 + h]))
                     if h % 2 == 0 else
                     (lambda e, h=h, kp_=kp_: e.tensor_scalar(out=kp_[:], in0=k.ps[2][:, h * 128:(h + 1) * 128], scalar1=E[:, 4 + h:5 + h], scalar2=None,
                                                            op0=ALU.mult)),
                     reads=[k.psb[2], b_E], writes=[bkp])
                S.op("pe", lambda e, h=h, kp_=kp_: e.matmul(k.ps[6][:, 0:129], lhsT=kp_[:], rhs=VA[:, h, 0:129], start=True, stop=True),
                     reads=[bkp, b_VA], writes=[k.psb[6]])
                S.op("dve", lambda e, h=h: e.tensor_scalar(out=C[h][:, 0:129], in0=C[h][:, 0:129], scalar1=E[:, 12 + h:13 + h], scalar2=None, op0=ALU.mult),
                     reads=[b_C[h], b_E], writes=[b_C[h]])
                S.op("dve", lambda e, h=h: e.scalar_tensor_tensor(out=C[h][:, 0:129], in0=k.ps[6][:, 0:129], scalar=float(128.0 ** -0.5), in1=C[h][:, 0:129],
                                                                 op0=ALU.mult, op1=ALU.add),
                     reads=[k.psb[6], b_C[h]], writes=[b_C[h]])
                S.op("act", lambda e, h=h: e.activation(out=Cb[h][:, 0:129], in_=C[h][:, 0:129], func=AF.Copy), reads=[b_C[h]], writes=[b_Cb[h]])
            for h in range(4):
                p, hf = h // 2, h % 2
                r0 = hf * 64
                if need_out:
                    sb_, bsb = Sb[si % 2], b_Sb[si % 2]
                    si += 1
                    S.op("pe", lambda e, p=p, r0=r0: e.matmul(k.ps[0][:, 0:128], lhsT=FT[r0:r0 + 64, 10 + p, :], rhs=FT[r0:r0 + 64, 8 + p, :],
                                                              start=True, stop=True), reads=[b_FT], writes=[k.psb[0]])
                    S.op("dve", lambda e, h=h, sb_=sb_: e.tensor_tensor(out=sb_[:], in0=k.ps[0][:, 0:128], in1=c.dj[d][h][:], op=ALU.mult),
                         reads=[k.psb[0], c.b], writes=[bsb])
                    S.op("pe", lambda e, h=h, sb_=sb_: e.matmul(k.ps[1][:, 0:128], lhsT=sb_[:], rhs=RV[:, h, :], start=True, stop=False),
                         reads=[bsb, b_RV], writes=[k.psb[1]], inc=False)
                    S.op("pe", lambda e, p=p, r0=r0: e.matmul(k.ps[1][:, 0:128], lhsT=FT[r0:r0 + 64, 8 + p, :], rhs=Spb[p][r0:r0 + 64, :],
                                                              start=False, stop=True), reads=[b_FT, b_Spb[p]], writes=[k.psb[1]])
                    S.op("act", lambda e, h=h, H_=H_: e.activation(out=H_[:, 512 + h * 128:512 + (h + 1) * 128], in_=k.ps[1][:, 0:128], func=AF.Identity,
                                                                  scale=c.rcol[:, 8 + d * 4 + h:8 + d * 4 + h + 1]),
                         reads=[k.psb[1], c.b], writes=[bH])
                S.op("dve", lambda e, h=h, p=p, hf=hf, r0=r0: e.tensor_scalar(out=Kpad[p][hf][:, r0:r0 + 64], in0=k.ps[4][:, h * 64:(h + 1) * 64],
                                                                            scalar1=c.rcol[:, 16 + d * 4 + h:16 + d * 4 + h + 1], scalar2=0.125,
                                                                            op0=ALU.mult, op1=ALU.mult),
                     reads=[k.psb[4], c.b], writes=[b_Kpad[p][hf]])
                if hf == 1:
                    S.op("pe", lambda e, p=p: e.matmul(k.ps[6][:, 0:128], lhsT=Kpad[p][0][:], rhs=RV[:, 2 * p, :], start=True, stop=False),
                         reads=[b_Kpad[p][0], b_RV], writes=[k.psb[6]], inc=False)
                    S.op("pe", lambda e, p=p: e.matmul(k.ps[6][:, 0:128], lhsT=Kpad[p][1][:], rhs=RV[:, 2 * p + 1, :], start=False, stop=True),
                         reads=[b_Kpad[p][1], b_RV], writes=[k.psb[6]])
                    S.op("dve", lambda e, p=p: e.scalar_tensor_tensor(out=Sp[p][:], in0=Sp[p][:], scalar=c.glp[:, d * 2 + p:d * 2 + p + 1], in1=k.ps[6][:, 0:128],
                                                                     op0=ALU.mult, op1=ALU.add),
                         reads=[b_Sp[p], c.b, k.psb[6]], writes=[b_Sp[p]])
                    S.op("act", lambda e, p=p: e.activation(out=Spb[p][:], in_=Sp[p][:], func=AF.Copy), reads=[b_Sp[p]], writes=[b_Spb[p]])
            if not need_out:
                continue
            if d == 0:
                S.dma("sp", k.H0d[t * 128:(t + 1) * 128, :], H_[:], reads=[bH], writes=[k.H0b[t]])
                continue
            tm(2, RC["mo"], 512)
            tm(3, RC["rg"], 512)
            S.op("act", lambda e: e.activation(out=gsig[:, 0:512], in_=k.ps[2][:, :], func=AF.Sigmoid), reads=[k.psb[2]], writes=[b_gsig])
            S.op("act", lambda e: e.activation(out=gsig[:, 512:1024], in_=k.ps[3][:, :], func=AF.Silu), reads=[k.psb[3]], writes=[b_gsig])
            h0_, bh0 = H0[ci % 2], b_H0[ci % 2]
            S.dma("sp", h0_[:], k.H0d[t * 128:(t + 1) * 128, :], reads=[k.H0b[t]], writes=[bh0])
            S.op("pool", lambda e, H_=H_, h0_=h0_: e.tensor_tensor(out=H_[:], in0=H_[:], in1=h0_[:], op=ALU.add), reads=[bH, bh0], writes=[bH])
            for blk in range(8):
                S.op("dve", lambda e, blk=blk, H_=H_: e.bn_stats(out=stt[:, blk, :], in_=H_[:, blk * 128:(blk + 1) * 128]), reads=[bH], writes=[b_st])
            for blk in range(8):
                S.op("dve", lambda e, blk=blk: e.bn_aggr(out=mv8[:, blk, :], in_=stt[:, blk, :]), reads=[b_st], writes=[b_st])
            S.op("act", lambda e: e.activation(out=sc8[:, 0:8], in_=mv8[:, :, 1], func=AF.Sqrt, bias=float(EPS), scale=1.0), reads=[b_st], writes=[b_st])
            S.op("dve", lambda e: e.reciprocal(out=sc8[:, 8:16], in_=sc8[:, 0:8]), reads=[b_st], writes=[b_st])
            S.op("dve", lambda e: e.scalar_tensor_tensor(out=sc8[:, 16:24], in0=mv8[:, :, 0], scalar=-1.0, in1=sc8[:, 8:16], op0=ALU.mult, op1=ALU.mult),
                 reads=[b_st], writes=[b_st])
            for blk in range(8):
                S.op("act", lambda e, blk=blk, H_=H_: e.activation(out=H_[:, blk * 128:(blk + 1) * 128], in_=H_[:, blk * 128:(blk + 1) * 128], func=AF.Identity,
                                                                  bias=sc8[:, 16 + blk:17 + blk], scale=sc8[:, 8 + blk:9 + blk]),
                     reads=[bH, b_st], writes=[bH])
            S.op("pool", lambda e, H_=H_: e.tensor_tensor(out=H_[:], in0=H_[:], in1=c.gg[:], op=ALU.mult), reads=[bH, c.b], writes=[bH])
            S.op("pool", lambda e, H_=H_: e.tensor_tensor(out=H_[:, 512:1024], in0=H_[:, 512:1024], in1=c.rb[:], op=ALU.add), reads=[bH, c.b], writes=[bH])
            S.op("dve", lambda e, H_=H_: e.tensor_tensor(out=H_[:], in0=H_[:], in1=gsig[:], op=ALU.mult), reads=[bH, b_gsig], writes=[bH])
            transpose_modulate(k, None, H_, bH, 0, zT, b_zT, 0, tpi, banks=(6,))
            xr_, bxr = xr[ci % 2], b_xr[ci % 2]
            ot_, bot = ot[ci % 2], b_ot[ci % 2]
            S.dma("sp", xr_[:], k.X[t * 128:(t + 1) * 128, :], reads=[k.Xb[t]], writes=[bxr])
            for h2 in range(2):
                for fc in range(8):
                    S.op("pe", lambda e, fc=fc, h2=h2: e.matmul(k.ps[7][:, :], lhsT=zT[:, fc, :], rhs=wo[:, fc, h2 * 512:(h2 + 1) * 512],
                                                               start=(fc == 0), stop=(fc == 7)),
                         reads=[b_zT, b_wo], writes=[k.psb[7]], inc=(fc == 7))
                pn_part(k, m, 0, 1.0, h2, k.ps[7][:, :], k.psb[7], ot_, bot)
            pn_finish(k, m, xr_, bxr, ot_, bot, small[ci % 2], b_small[ci % 2], k.X[t * 128:(t + 1) * 128, :], [k.Xb[t]])
        S.barrier()


_CACHE = {}


def _consts():
    c = {"ident": np.eye(128, dtype=np.float32)}
    perm = np.zeros((128, 128), np.float32)
    for d in range(128):
        if (d % 32) < 16:
            perm[d + 16, d] = -1.0
        else:
            perm[d - 16, d] = 1.0
    c["perm"] = perm
    kl = np.arange(128)[:, None]
    ql = np.arange(128)[None, :]
    c["maskp"] = np.tile((ql <= kl).astype(np.float32), (1, 4))
    c["maskn"] = np.tile((kl <= ql).astype(np.float32), (1, 4))
    t = np.arange(T)
    pos = np.stack([t // 64, t % 64], axis=0).astype(np.float32)
    inv = (10000.0 ** (-np.arange(16, dtype=np.float32) / 16.0)).astype(np.float32)
    d = np.arange(128) % 64
    ang = (pos[d // 32, :] * inv[d % 16][:, None]).astype(np.float32)
    c["ropec"] = np.cos(ang).astype(np.float32)
    c["ropes"] = np.sin(ang).astype(np.float32)
    jj = np.arange(128)
    tri0 = (jj[:, None] <= jj[None, :]).astype(np.float32)
    tri1 = (jj[:, None] >= jj[None, :]).astype(np.float32)
    c["tri"] = np.stack([tri0, tri1])
    c["mbm"] = -c["tri"]
    c["mwm"] = -(1.0 - c["tri"])
    c["mall"] = -np.ones((128, 128), np.float32)
    pos = np.zeros((128, 8), np.float32)
    L = 128
    pos[:, 0] = -(jj + 1)
    pos[:, 1] = -(L - jj)
    pos[:, 2] = jj + 1
    pos[:, 3] = L - jj
    pos[:, 4] = L - 1 - jj
    pos[:, 5] = jj
    c["posc"] = pos
    return c


def kernel(**inputs):
    stop_after = inputs.pop("_stop_after", None)
    f = lambda a: np.ascontiguousarray(np.asarray(a, dtype=np.float32))
    x = f(inputs["x"])
    B = x.shape[0]
    key = stop_after
    if key not in _CACHE:
        _CACHE[key] = build_nc(stop_after)
    nc = _CACHE[key]
    shared = {n: f(inputs[n]) for n in ("ada_w", "ada_b", "ln_g", "ln_b", "ffn_w_in", "ffn_w_out", "attn_w_in", "attn_w_out",
                                         "diff_lambda", "diff_norm_g", "sink_logits", "rec_w_in", "rec_w_out",
                                         "mlstm_gate_b", "mlstm_norm_g", "ret_decay_logit", "ret_norm_g", "ret_norm_b")}
    shared.update(_consts())
    c = f(inputs["c"])
    ctx = f(inputs["ctx"])
    c_ctx = f(inputs["c_ctx"])
    in_maps = []
    for b in range(B):
        d = dict(shared)
        d["x"] = x[b]
        d["ctx"] = ctx[b]
        d["cc"] = np.concatenate([c[b].reshape(8, 128), c_ctx.reshape(8, 128)], axis=0)
        in_maps.append(d)
    res = run_bass_kernel_spmd(nc, in_maps, core_ids=list(range(B)))
    kernel.last_dbg = res.results[0].get("dbg")
    if kernel.last_dbg is not None:
        np.save("last_dbg.npy", kernel.last_dbg)
    return np.stack([res.results[b]["out"] for b in range(B)], axis=0).astype(np.float32)
```

```python
import math
import os
from contextlib import ExitStack
import numpy as np
import concourse.bass as bass
import concourse.mybir as mybir
from concourse.bass_utils import run_bass_kernel_spmd

F32 = mybir.dt.float32
BF16 = mybir.dt.bfloat16
ALU = mybir.AluOpType
AF = mybir.ActivationFunctionType

D = 1024
T = 4096
TC = 256
NT = (T + TC) // 128
NTX = T // 128
DFF = 2816
NJ = DFF // 128
DEPTH = 2
ALPHA = (2.0 * DEPTH) ** 0.25
EPS = 1e-5
ATTN_IN = 2304
REC_IN = 3600


class Buf:
    __slots__ = ("name", "w", "r")

    def __init__(self, name=""):
        self.name = name
        self.w = None
        self.r = {}


class Sched:
    ENG = ("pe", "act", "dve", "pool", "sp")

    def __init__(self, nc, es, n_dma_sems=10):
        self.nc = nc
        self.engs = {"pe": nc.tensor, "act": nc.scalar, "dve": nc.vector, "pool": nc.gpsimd, "sp": nc.sync}
        self.sems = []
        self.cnt = []
        self.esem = {}
        for e in self.ENG:
            self.esem[e] = self._newsem(es, "c_" + e)
        self.dsem = {q: [self._newsem(es, "d_%s%d" % (q, i)) for i in range(n_dma_sems)] for q in ("sp", "pool")}
        self.dptr = {q: 0 for q in self.dsem}
        self.seen = {e: {} for e in self.ENG}
        self.nwait = 0
        self.nins = 0
        self.log = {e: [] for e in self.ENG}

    def _newsem(self, es, name):
        self.sems.append(es.enter_context(self.nc.semaphore(name)))
        self.cnt.append(0)
        return len(self.sems) - 1

    def _waits(self, e, reads, writes):
        need = {}
        own = self.esem[e]
        for b in reads:
            if b.w is not None:
                s, v = b.w
                if need.get(s, 0) < v:
                    need[s] = v
        for b in writes:
            if b.w is not None:
                s, v = b.w
                if s != own and need.get(s, 0) < v:
                    need[s] = v
            for s, v in b.r.items():
                if s != own and need.get(s, 0) < v:
                    need[s] = v
        seen = self.seen[e]
        eng = self.engs[e]
        for s, v in need.items():
            if seen.get(s, 0) >= v:
                continue
            eng.wait_ge(self.sems[s], v)
            self.log[e].append(("w", s, v))
            self.nwait += 1
            seen[s] = v

    def _mark(self, tok, reads, writes):
        s, v = tok
        for b in reads:
            if b.r.get(s, 0) < v:
                b.r[s] = v
        for b in writes:
            b.w = tok
            b.r = {}

    def op(self, e, fn, reads=(), writes=(), inc=True):
        self._waits(e, reads, writes)
        ins = fn(self.engs[e])
        self.nins += 1
        s = self.esem[e]
        if inc:
            self.cnt[s] += 1
            ins.then_inc(self.sems[s], 1)
            tok = (s, self.cnt[s])
            self.log[e].append(("i", s, 1))
        else:
            tok = (s, self.cnt[s] + 1)
        self._mark(tok, reads, writes)
        return tok

    def dma(self, q, out, in_, reads=(), writes=(), **kw):
        lst = self.dsem[q]
        i = self.dptr[q]
        self.dptr[q] = (i + 1) % len(lst)
        s = lst[i]
        eng = self.engs[q]
        if self.cnt[s] > 0 and self.seen[q].get(s, 0) < self.cnt[s]:
            eng.wait_ge(self.sems[s], self.cnt[s])
            self.log[q].append(("w", s, self.cnt[s]))
            self.seen[q][s] = self.cnt[s]
        self._waits(q, reads, writes)
        ins = eng.dma_start(out=out, in_=in_, **kw)
        self.nins += 1
        self.cnt[s] += 16
        ins.then_inc(self.sems[s], 16)
        self.log[q].append(("i", s, 16))
        tok = (s, self.cnt[s])
        self._mark(tok, reads, writes)
        return tok

    def barrier(self):
        for e in self.ENG:
            eng = self.engs[e]
            seen = self.seen[e]
            for s in range(len(self.sems)):
                v = self.cnt[s]
                if v > 0 and seen.get(s, 0) < v and s != self.esem[e]:
                    eng.wait_ge(self.sems[s], v)
                    self.log[e].append(("w", s, v))
                    seen[s] = v

    def finish(self):
        eng = self.engs["sp"]
        seen = self.seen["sp"]
        for s in range(len(self.sems)):
            v = self.cnt[s]
            if v > 0 and seen.get(s, 0) < v:
                eng.wait_ge(self.sems[s], v)
                self.log["sp"].append(("w", s, v))
                seen[s] = v

    def check_deadlock(self):
        val = [0] * len(self.sems)
        pos = {e: 0 for e in self.ENG}
        progress = True
        while progress:
            progress = False
            for e in self.ENG:
                lg = self.log[e]
                i = pos[e]
                while i < len(lg):
                    kind, s, v = lg[i]
                    if kind == "w":
                        if val[s] < v:
                            break
                    else:
                        val[s] += v
                    i += 1
                if i != pos[e]:
                    progress = True
                    pos[e] = i
        stuck = {e: (pos[e], len(self.log[e]), self.log[e][pos[e]]) for e in self.ENG if pos[e] < len(self.log[e])}
        return stuck


class K:
    pass


_UID = [0]


def _sb(k, es, name, shape, dt):
    _UID[0] += 1
    t = es.enter_context(k.nc.sbuf_tensor("%s_%d" % (name, _UID[0]), list(shape), dt))
    return t


def build_nc(stop_after=None):
    nc = bass.Bass("TRN2", target_bir_lowering=False)
    k = K()
    k.nc = nc
    dram = lambda name, shape, dt=F32, kind="ExternalInput": nc.dram_tensor(name, list(shape), dt, kind=kind).ap()
    k.x_in = dram("x", [T, D])
    k.ctx_in = dram("ctx", [TC, D])
    k.cc_in = dram("cc", [16, 128])
    k.ada_w = dram("ada_w", [DEPTH, D, 9 * D])
    k.ada_b = dram("ada_b", [DEPTH, 9 * D])
    k.ln_g = dram("ln_g", [DEPTH, 3, D])
    k.ln_b = dram("ln_b", [DEPTH, 3, D])
    k.ffn_w_in = dram("ffn_w_in", [DEPTH, 2, D, 2 * DFF])
    k.ffn_w_out = dram("ffn_w_out", [DEPTH, 2, DFF, D])
    k.ident_in = dram("ident", [128, 128])
    k.attn_w_in = dram("attn_w_in", [1, D, ATTN_IN])
    k.attn_w_out = dram("attn_w_out", [1, D, D])
    k.diff_lambda = dram("diff_lambda", [1, 4, 64])
    k.diff_norm_g = dram("diff_norm_g", [1, 128])
    k.sink_logits = dram("sink_logits", [1, 8])
    k.rec_w_in = dram("rec_w_in", [1, D, REC_IN])
    k.rec_w_out = dram("rec_w_out", [1, D, D])
    k.mlstm_gate_b = dram("mlstm_gate_b", [1, 2, 2, 4])
    k.mlstm_norm_g = dram("mlstm_norm_g", [1, 512])
    k.ret_decay_logit = dram("ret_decay_logit", [1, 2, 4])
    k.ret_norm_g = dram("ret_norm_g", [1, 512])
    k.ret_norm_b = dram("ret_norm_b", [1, 512])
    k.tri_in = dram("tri", [2, 128, 128])
    k.mb_in = dram("mbm", [2, 128, 128])
    k.mw_in = dram("mwm", [2, 128, 128])
    k.mall_in = dram("mall", [128, 128])
    k.pos_in = dram("posc", [128, 8])
    k.perm_in = dram("perm", [128, 128])
    k.maskp_in = dram("maskp", [128, 512])
    k.maskn_in = dram("maskn", [128, 512])
    k.ropec_in = dram("ropec", [128, T])
    k.ropes_in = dram("ropes", [128, T])
    k.out = dram("out", [T, D], kind="ExternalOutput")
    k.dbg_on = bool(os.environ.get("KDBG"))
    if k.dbg_on:
        k.dbg = dram("dbg", [NT * 128, D], kind="ExternalOutput")
        k.BBd = nc.dram_tensor("BBd", [NT * 128, 512], F32).ap()
        k.BBb = [Buf() for _ in range(NT)]
    k.X = nc.dram_tensor("Xs", [NT * 128, D], F32).ap()
    k.MODROW = nc.dram_tensor("modrow", [DEPTH, 2, 9 * D], F32).ap()
    k.Xb = [Buf("X%d" % t) for t in range(NT)]
    k.ABd = nc.dram_tensor("ABd", [NT * 128, 512], F32).ap()
    k.ABb = [Buf("AB%d" % t) for t in range(NT)]
    k.H0d = nc.dram_tensor("H0d", [T, D], F32).ap()
    k.H0b = [Buf("H0%d" % t) for t in range(NTX)]
    k.modrow_b = [Buf("modrow%d" % l) for l in range(DEPTH)]
    k.stop_after = stop_after

    with ExitStack() as es:
        S = Sched(nc, es)
        k.S = S
        k.ps = []
        k.psb = []
        for i in range(8):
            k.ps.append(es.enter_context(nc.psum_tensor("ps%d" % i, [128, 512], F32)))
            k.psb.append(Buf("ps%d" % i))
        k.ident = _sb(k, es, "ident", [128, 128], F32)
        k.ident_b = Buf("ident")
        S.dma("sp", k.ident[:], k.ident_in[:, :], writes=[k.ident_b])
        emit_program(k)
        S.finish()
        stuck = S.check_deadlock()
        print("instructions", S.nins, "waits", S.nwait, "sem counts", S.cnt[:5], "DEADLOCK" if stuck else "no-deadlock", stuck, flush=True)
    return nc


def emit_program(k):
    phase_mod(k)
    src0 = lambda t: (k.x_in[t * 128:(t + 1) * 128, :] if t < NTX else k.ctx_in[(t - NTX) * 128:(t - NTX + 1) * 128, :])
    phase_ffn(k, 0, 0, 0, src0, NT)
    if k.stop_after == "ffn00":
        return copy_out(k)
    with ExitStack() as es:
        load_attn_consts(k, es)
        if k.stop_after == "attn_c":
            k.S.barrier()
            return copy_out(k)
        phase_attn_a(k)
        if k.stop_after in ("attn_a", "attn_ai"):
            return copy_out(k)
        phase_attn_b(k)
    if k.stop_after == "mix0":
        return copy_out(k)
    srcX = lambda t: k.X[t * 128:(t + 1) * 128, :]
    phase_ffn(k, 0, 1, 2, srcX, NT)
    if k.stop_after == "ffn01":
        return copy_out(k)
    phase_ffn(k, 1, 0, 0, srcX, NT)
    if k.stop_after == "ffn10":
        return copy_out(k)
    with ExitStack() as es:
        load_rec_consts(k, es)
        if k.stop_after == "rec_c":
            k.S.barrier()
            return copy_out(k)
        phase_rec(k, 0)
        if k.stop_after == "rec0":
            return copy_out(k)
        phase_rec(k, 1)
    if k.stop_after == "mix1":
        return copy_out(k)
    phase_ffn(k, 1, 1, 2, srcX, NTX)
    copy_out(k)


def copy_out(k):
    S = k.S
    if k.dbg_on:
        for t in range(NT):
            S.dma("sp", k.dbg[t * 128:(t + 1) * 128, 0:512], k.ABd[t * 128:(t + 1) * 128, :], reads=[k.ABb[t]])
            S.dma("sp", k.dbg[t * 128:(t + 1) * 128, 512:1024], k.BBd[t * 128:(t + 1) * 128, :], reads=[k.BBb[t]])
    for t in range(0, NTX, 4):
        S.dma("sp", k.out[t * 128:(t + 4) * 128, :], k.X[t * 128:(t + 4) * 128, :],
              reads=[k.Xb[t + i] for i in range(4)])


def phase_mod(k):
    nc, S = k.nc, k.S
    with ExitStack() as es:
        cc = _sb(k, es, "cc", [16, 128], F32)
        ccs = _sb(k, es, "ccs", [16, 128], F32)
        scb = _sb(k, es, "scb", [128, 16], BF16)
        brow = _sb(k, es, "brow", [2, 9 * D], F32)
        mrow = _sb(k, es, "mrow", [2, 9 * D], F32)
        wb = [_sb(k, es, "wblk%d" % i, [128, 8, 512], BF16) for i in range(2)]
        b_cc, b_ccs, b_scb, b_brow, b_mrow = Buf(), Buf(), Buf(), Buf(), Buf()
        b_wb = [Buf(), Buf()]
        S.dma("sp", cc[:], k.cc_in[:, :], writes=[b_cc])
        S.op("act", lambda e: e.activation(out=ccs[:], in_=cc[:], func=AF.Silu), reads=[b_cc], writes=[b_ccs])
        tp = k.ps[6]
        S.op("pe", lambda e: e.transpose(out=tp[:, 0:16], in_=ccs[:], identity=k.ident[0:16, 0:16]),
             reads=[b_ccs, k.ident_b], writes=[k.psb[6]])
        S.op("dve", lambda e: e.tensor_copy(out=scb[:], in_=tp[:, 0:16]), reads=[k.psb[6]], writes=[b_scb])
        scb3 = scb[:].rearrange("p (v c) -> p v c", v=2)
        blk = 0
        for l in range(DEPTH):
            S.dma("sp", brow[:], k.ada_b[l, :].partition_broadcast(2), writes=[b_brow])
            wsrc = k.ada_w[l].rearrange("(c p) n -> p c n", p=128)
            for nb in range(18):
                w = wb[blk % 2]
                bw = b_wb[blk % 2]
                S.dma("pool", w[:], wsrc[:, :, nb * 512:(nb + 1) * 512], writes=[bw])
                pb = 4 + (blk % 2)
                for kc in range(8):
                    S.op("pe", lambda e, kc=kc, w=w, pb=pb: e.matmul(k.ps[pb][0:2, :], lhsT=scb3[:, :, kc], rhs=w[:, kc, :],
                                                                    start=(kc == 0), stop=(kc == 7)),
                         reads=[b_scb, bw], writes=[k.psb[pb]], inc=(kc == 7))
                S.op("dve", lambda e, pb=pb, nb=nb: e.tensor_tensor(out=mrow[:, nb * 512:(nb + 1) * 512], in0=k.ps[pb][0:2, :],
                                                                   in1=brow[:, nb * 512:(nb + 1) * 512], op=ALU.add),
                     reads=[k.psb[pb], b_brow], writes=[b_mrow])
                blk += 1
            S.dma("sp", k.MODROW[l], mrow[:], reads=[b_mrow], writes=[k.modrow_b[l]])
        S.barrier()


def load_mod(k, es, l, s, tag):
    nc, S = k.nc, k.S
    m = K()
    m.st = _sb(k, es, "mst" + tag, [32, 128], F32)
    m.mc = _sb(k, es, "mc" + tag, [128, 32], F32)
    m.gb = [_sb(k, es, "gb%d" % v + tag, [128, D], F32) for v in range(2)]
    m.lng = _sb(k, es, "lng" + tag, [128, D], F32)
    m.lnb = _sb(k, es, "lnb" + tag, [128, D], F32)
    m.b_st, m.b_mc, m.b_gb, m.b_ln = Buf(), Buf(), Buf(), Buf()
    for j in range(2):
        for v in range(2):
            r0 = (j * 2 + v) * 8
            src = k.MODROW[l, v, (s * 3 + j) * D:(s * 3 + j + 1) * D].rearrange("(c p) -> c p", p=128)
            S.dma("sp", m.st[r0:r0 + 8, :], src, reads=[k.modrow_b[l]], writes=[m.b_st])
    tp = k.ps[6]
    S.op("pe", lambda e: e.transpose(out=tp[:, 0:32], in_=m.st[:], identity=k.ident[0:32, 0:32]),
         reads=[m.b_st, k.ident_b], writes=[k.psb[6]])
    S.op("dve", lambda e: e.tensor_copy(out=m.mc[:, 0:16], in_=tp[:, 0:16]), reads=[k.psb[6]], writes=[m.b_mc])
    S.op("dve", lambda e: e.tensor_scalar_add(out=m.mc[:, 16:32], in0=tp[:, 16:32], scalar1=1.0),
         reads=[k.psb[6]], writes=[m.b_mc])
    for v in range(2):
        S.dma("sp", m.gb[v][:], k.MODROW[l, v, (s * 3 + 2) * D:(s * 3 + 3) * D].partition_broadcast(128),
              reads=[k.modrow_b[l]], writes=[m.b_gb])
    S.dma("sp", m.lng[:], k.ln_g[l, s, :].partition_broadcast(128), writes=[m.b_ln])
    S.dma("sp", m.lnb[:], k.ln_b[l, s, :].partition_broadcast(128), writes=[m.b_ln])
    m.shift = lambda fc, v: m.mc[:, (0 * 2 + v) * 8 + fc:(0 * 2 + v) * 8 + fc + 1]
    m.scale = lambda fc, v: m.mc[:, (1 * 2 + v) * 8 + fc:(1 * 2 + v) * 8 + fc + 1]
    return m


def transpose_modulate(k, m, xt, b_xt, v, hT, b_hT, col0, tpi, banks=(6, 7), by_half=False):
    S = k.S
    for half in range(2):
        pb = banks[tpi[0] % len(banks)]
        tpi[0] += 1
        tp = k.ps[pb]
        for q in range(4):
            fc = half * 4 + q
            S.op("pe", lambda e, fc=fc, q=q, tp=tp: e.transpose(out=tp[:, q * 128:(q + 1) * 128],
                                                               in_=xt[:, fc * 128:(fc + 1) * 128], identity=k.ident[:]),
                 reads=[b_xt, k.ident_b], writes=[k.psb[pb]], inc=(q == 3))
        for q in range(4):
            fc = half * 4 + q
            dst = hT[:, fc, col0:col0 + 128]
            src = tp[:, q * 128:(q + 1) * 128]
            bh = b_hT[fc] if isinstance(b_hT, list) else b_hT
            on_act = (half == 0) if by_half else (q % 2 == 0)
            if m is None:
                if on_act:
                    S.op("act", lambda e, dst=dst, src=src: e.activation(out=dst, in_=src, func=AF.Copy),
                         reads=[k.psb[pb]], writes=[bh])
                else:
                    S.op("dve", lambda e, dst=dst, src=src: e.tensor_copy(out=dst, in_=src), reads=[k.psb[pb]], writes=[bh])
            elif on_act:
                S.op("act", lambda e, fc=fc, dst=dst, src=src: e.activation(out=dst, in_=src, func=AF.Identity,
                                                                           bias=m.shift(fc, v), scale=m.scale(fc, v)),
                     reads=[k.psb[pb], m.b_mc], writes=[bh])
            else:
                S.op("dve", lambda e, fc=fc, dst=dst, src=src: e.tensor_scalar(out=dst, in0=src, scalar1=m.scale(fc, v),
                                                                              scalar2=m.shift(fc, v), op0=ALU.mult, op1=ALU.add),
                     reads=[k.psb[pb], m.b_mc], writes=[bh])


def pn_part(k, m, v, weight, h, yp, yb, ot, b_ot):
    k.S.op("dve", lambda e: e.scalar_tensor_tensor(out=ot[:, h * 512:(h + 1) * 512], in0=yp, scalar=float(weight),
                                                  in1=m.gb[v][:, h * 512:(h + 1) * 512], op0=ALU.mult, op1=ALU.mult),
           reads=[yb, m.b_gb], writes=[b_ot])


def pn_finish(k, m, xr, b_xr, ot, b_ot, small, b_small, dst, dst_bufs, pool_ok=True):
    S = k.S
    S.op("dve", lambda e: e.scalar_tensor_tensor(out=xr[:], in0=xr[:], scalar=float(ALPHA), in1=ot[:], op0=ALU.mult, op1=ALU.add),
         reads=[b_xr, b_ot], writes=[b_xr])
    layer_norm_rows(k, xr, b_xr, ot, b_ot, small, b_small)
    eng = "pool" if pool_ok else "dve"
    S.op(eng, lambda e: e.tensor_tensor(out=ot[:], in0=ot[:], in1=m.lng[:], op=ALU.mult), reads=[b_ot, m.b_ln], writes=[b_ot])
    S.op(eng, lambda e: e.tensor_tensor(out=ot[:], in0=ot[:], in1=m.lnb[:], op=ALU.add), reads=[b_ot, m.b_ln], writes=[b_ot])
    S.dma("sp", dst, ot[:], reads=[b_ot], writes=dst_bufs)


def post_norm(k, m, v, weight, ypairs, xr, b_xr, ot, b_ot, small, b_small, dst, dst_bufs, pool_ok=True):
    for h, (yp, yb) in enumerate(ypairs):
        pn_part(k, m, v, weight, h, yp, yb, ot, b_ot)
    pn_finish(k, m, xr, b_xr, ot, b_ot, small, b_small, dst, dst_bufs, pool_ok)


def layer_norm_rows(k, src, b_src, ot, b_ot, small, b_small):
    S = k.S
    st = small[:, 0:12]
    mv = small[:, 12:14]
    sd = small[:, 14:15]
    rstd = small[:, 15:16]
    nmr = small[:, 16:17]
    for h in range(2):
        S.op("dve", lambda e, h=h: e.bn_stats(out=small[:, h * 6:(h + 1) * 6], in_=src[:, h * 512:(h + 1) * 512]),
             reads=[b_src], writes=[b_small])
    S.op("dve", lambda e: e.bn_aggr(out=mv, in_=st), reads=[b_small], writes=[b_small])
    S.op("act", lambda e: e.activation(out=sd, in_=small[:, 13:14], func=AF.Sqrt, bias=float(EPS), scale=1.0),
         reads=[b_small], writes=[b_small])
    S.op("dve", lambda e: e.reciprocal(out=rstd, in_=sd), reads=[b_small], writes=[b_small])
    S.op("dve", lambda e: e.tensor_scalar(out=nmr, in0=small[:, 12:13], scalar1=rstd, scalar2=-1.0, op0=ALU.mult, op1=ALU.mult),
         reads=[b_small], writes=[b_small])
    S.op("act", lambda e: e.activation(out=ot[:], in_=src[:], func=AF.Identity, bias=nmr, scale=rstd),
         reads=[b_src, b_small], writes=[b_ot])


def phase_ffn(k, l, half, s, src_fn, ntiles):
    nc, S = k.nc, k.S
    with ExitStack() as es:
        w1 = _sb(k, es, "w1", [128, 8, 2 * DFF], BF16)
        w2 = _sb(k, es, "w2", [128, NJ, D], BF16)
        b_w1, b_w2 = Buf(), Buf()
        w1src = k.ffn_w_in[l, half].rearrange("(c p) n -> p c n", p=128)
        for c0 in range(0, 2 * DFF, 1408):
            for kc in range(8):
                S.dma("pool", w1[:, kc, c0:c0 + 1408], w1src[:, kc, c0:c0 + 1408], writes=[b_w1])
        w2src = k.ffn_w_out[l, half].rearrange("(c p) n -> p c n", p=128)
        for jc in range(NJ):
            S.dma("pool", w2[:, jc, :], w2src[:, jc, :], writes=[b_w2])
        m = load_mod(k, es, l, s, "f")
        hT = _sb(k, es, "hT", [128, 8, 512], BF16)
        aT = _sb(k, es, "aT", [128, NJ, 512], BF16)
        b_hT, b_aT = [Buf() for _ in range(8)], Buf()
        xt = [_sb(k, es, "xt%d" % i, [128, D], F32) for i in range(2)]
        xt2 = [_sb(k, es, "xr%d" % i, [128, D], F32) for i in range(2)]
        ot = [_sb(k, es, "ot%d" % i, [128, D], F32) for i in range(2)]
        sg = [_sb(k, es, "sg%d" % i, [128, 512], BF16) for i in range(2)]
        small = [_sb(k, es, "sm%d" % i, [128, 32], F32) for i in range(2)]
        b_xt, b_xt2, b_ot, b_sg, b_small = ([Buf(), Buf()] for _ in range(5))
        groups = []
        t = 0
        while t < min(ntiles, NTX):
            groups.append((list(range(t, min(t + 4, NTX))), 0))
            t += 4
        if ntiles > NTX:
            groups.append((list(range(NTX, ntiles)), 1))
        tpi = [0]
        xi = 0
        ri = 0
        gi = 0
        for tiles, v in groups:
            n = len(tiles) * 128
            for i, t in enumerate(tiles):
                x_ = xt[xi % 2]
                bx = b_xt[xi % 2]
                xi += 1
                S.dma("sp", x_[:], src_fn(t), reads=[k.Xb[t]], writes=[bx])
                transpose_modulate(k, m, x_, bx, v, hT, b_hT, i * 128, tpi, by_half=True)
            for jc in range(NJ):
                pg = gi % 2
                pu = 2 + gi % 2
                gi += 1
                for kc in range(8):
                    S.op("pe", lambda e, kc=kc, jc=jc, pg=pg: e.matmul(k.ps[pg][:, 0:n], lhsT=w1[:, kc, jc * 128:(jc + 1) * 128],
                                                                      rhs=hT[:, kc, 0:n], start=(kc == 0), stop=(kc == 7)),
                         reads=[b_w1, b_hT[kc]], writes=[k.psb[pg]], inc=(kc == 7))
                for kc in range(8):
                    S.op("pe", lambda e, kc=kc, jc=jc, pu=pu: e.matmul(k.ps[pu][:, 0:n], lhsT=w1[:, kc, DFF + jc * 128:DFF + (jc + 1) * 128],
                                                                      rhs=hT[:, kc, 0:n], start=(kc == 0), stop=(kc == 7)),
                         reads=[b_w1, b_hT[kc]], writes=[k.psb[pu]], inc=(kc == 7))
                sg_ = sg[jc % 2]
                bs = b_sg[jc % 2]
                S.op("act", lambda e, pg=pg, sg_=sg_: e.activation(out=sg_[:, 0:n], in_=k.ps[pg][:, 0:n], func=AF.Silu),
                     reads=[k.psb[pg]], writes=[bs])
                S.op("dve", lambda e, pu=pu, sg_=sg_, jc=jc: e.tensor_tensor(out=aT[:, jc, 0:n], in0=sg_[:, 0:n], in1=k.ps[pu][:, 0:n],
                                                                           op=ALU.mult),
                     reads=[bs, k.psb[pu]], writes=[b_aT])
            for i, t in enumerate(tiles):
                for h in range(2):
                    for jc in range(NJ):
                        S.op("pe", lambda e, jc=jc, h=h, i=i: e.matmul(k.ps[4 + h][:, :], lhsT=aT[:, jc, i * 128:(i + 1) * 128],
                                                                      rhs=w2[:, jc, h * 512:(h + 1) * 512],
                                                                      start=(jc == 0), stop=(jc == NJ - 1)),
                             reads=[b_aT, b_w2], writes=[k.psb[4 + h]], inc=(jc == NJ - 1))
                r_ = xt2[ri % 2]
                br = b_xt2[ri % 2]
                o_ = ot[ri % 2]
                bo = b_ot[ri % 2]
                sm = small[ri % 2]
                bsm = b_small[ri % 2]
                ri += 1
                S.dma("sp", r_[:], src_fn(t), reads=[k.Xb[t]], writes=[br])
                post_norm(k, m, v, 0.5, [(k.ps[4][:, :], k.psb[4]), (k.ps[5][:, :], k.psb[5])], r_, br, o_, bo, sm, bsm,
                          k.X[t * 128:(t + 1) * 128, :], [k.Xb[t]])
        S.barrier()


def token_groups(ntiles=NT):
    groups = []
    t = 0
    while t < min(ntiles, NTX):
        groups.append((list(range(t, min(t + 4, NTX))), 0))
        t += 4
    if ntiles > NTX:
        groups.append((list(range(NTX, ntiles)), 1))
    return groups


def rope_evac(k, ps_ap, ps_buf, psp_ap, psp_buf, n, dst_ap, b_dst, rc, rs, b_rt, W):
    S = k.S
    S.op("dve", lambda e: e.tensor_tensor(out=W.t1[:, 0:n], in0=ps_ap, in1=rc[:, 0:n], op=ALU.mult),
         reads=[ps_buf, b_rt], writes=[W.b_t1])
    S.op("dve", lambda e: e.tensor_tensor(out=W.t2[:, 0:n], in0=psp_ap, in1=rs[:, 0:n], op=ALU.mult),
         reads=[psp_buf, b_rt], writes=[W.b_t2])
    S.op("dve", lambda e: e.tensor_tensor(out=dst_ap, in0=W.t1[:, 0:n], in1=W.t2[:, 0:n], op=ALU.add),
         reads=[W.b_t1, W.b_t2], writes=[b_dst])


def build_partner_weights(k, wP, b_wP, wX, b_wX, ncols):
    S = k.S
    g = ncols // 32
    for kc in range(8):
        src = wX[:, kc, 0:ncols].rearrange("p (g h f) -> p g h f", g=g, h=2)
        dst = wP[:, kc, 0:ncols].rearrange("p (g h f) -> p g h f", g=g, h=2)
        S.op("act", lambda e, src=src, dst=dst: e.mul(out=dst[:, :, 0, :], in_=src[:, :, 1, :], mul=-1.0), reads=[b_wX], writes=[b_wP])
        S.op("dve", lambda e, src=src, dst=dst: e.tensor_copy(out=dst[:, :, 1, :], in_=src[:, :, 0, :]), reads=[b_wX], writes=[b_wP])


def load_attn_consts(k, es):
    S = k.S
    k.perm = _sb(k, es, "perm", [128, 128], BF16)
    k.maskp = _sb(k, es, "maskp", [128, 512], BF16)
    k.maskn = _sb(k, es, "maskn", [128, 512], BF16)
    k.g08 = _sb(k, es, "g08", [128, 128], F32)
    k.esink = _sb(k, es, "esink", [128, 8], F32)
    k.dl = _sb(k, es, "dl", [128, 4, 64], F32)
    k.lsm = _sb(k, es, "lsm", [128, 2, 64], F32)
    k.lam = _sb(k, es, "lam", [128, 8], F32)
    k.b_cst = Buf()
    S.dma("pool", k.perm[:], k.perm_in[:, :], writes=[k.b_cst])
    S.dma("pool", k.maskp[:], k.maskp_in[:, :], writes=[k.b_cst])
    S.dma("pool", k.maskn[:], k.maskn_in[:, :], writes=[k.b_cst])
    S.dma("sp", k.g08[:], k.diff_norm_g[0, :].partition_broadcast(128), writes=[k.b_cst])
    S.dma("sp", k.esink[:], k.sink_logits[0, :].partition_broadcast(128), writes=[k.b_cst])
    S.dma("sp", k.dl[:].rearrange("p a b -> p (a b)"), k.diff_lambda[0].rearrange("a b -> (a b)").partition_broadcast(128),
          writes=[k.b_cst])
    S.op("dve", lambda e: e.tensor_scalar_mul(out=k.g08[:], in0=k.g08[:], scalar1=0.8), reads=[k.b_cst], writes=[k.b_cst])
    S.op("act", lambda e: e.activation(out=k.esink[:], in_=k.esink[:], func=AF.Exp), reads=[k.b_cst], writes=[k.b_cst])
    S.op("dve", lambda e: e.tensor_tensor(out=k.lsm[:, 0, :], in0=k.dl[:, 0, :], in1=k.dl[:, 1, :], op=ALU.mult),
         reads=[k.b_cst], writes=[k.b_cst])
    S.op("dve", lambda e: e.tensor_tensor(out=k.lsm[:, 1, :], in0=k.dl[:, 2, :], in1=k.dl[:, 3, :], op=ALU.mult),
         reads=[k.b_cst], writes=[k.b_cst])
    S.op("dve", lambda e: e.reduce_sum(out=k.lam[:, 0:2], in_=k.lsm[:], axis=mybir.AxisListType.X), reads=[k.b_cst], writes=[k.b_cst])
    S.op("act", lambda e: e.activation(out=k.lam[:, 2:4], in_=k.lam[:, 0:2], func=AF.Exp), reads=[k.b_cst], writes=[k.b_cst])
    S.op("dve", lambda e: e.tensor_tensor(out=k.lam[:, 4:5], in0=k.lam[:, 3:4], in1=k.lam[:, 2:3], op=ALU.subtract),
         reads=[k.b_cst], writes=[k.b_cst])
    S.op("dve", lambda e: e.tensor_scalar_add(out=k.lam[:, 5:6], in0=k.lam[:, 4:5], scalar1=-0.2), reads=[k.b_cst], writes=[k.b_cst])
    k.neglam = k.lam[:, 5:6]


def attn_inproj(k, es, m, wX, b_wX, wP, b_wP, specs, vspec, tag):
    S = k.S
    W = K()
    W.t1 = _sb(k, es, "t1" + tag, [128, 512], F32)
    W.t2 = _sb(k, es, "t2" + tag, [128, 512], F32)
    W.b_t1, W.b_t2 = Buf(), Buf()
    hT = _sb(k, es, "hT" + tag, [128, 8, 512], BF16)
    b_hT = Buf()
    xt = [_sb(k, es, "xt%d" % i + tag, [128, D], F32) for i in range(2)]
    b_xt = [Buf(), Buf()]
    rc = [_sb(k, es, "rc%d" % i + tag, [128, 512], F32) for i in range(2)]
    rs = [_sb(k, es, "rs%d" % i + tag, [128, 512], F32) for i in range(2)]
    b_rt = [Buf(), Buf()]
    tpi = [0]
    xi = pj = pp = vv = 0
    for gidx, (tiles, v) in enumerate(token_groups()):
        n = len(tiles) * 128
        tok0 = tiles[0] * 128
        for i, t in enumerate(tiles):
            x_ = xt[xi % 2]
            bx = b_xt[xi % 2]
            xi += 1
            S.dma("sp", x_[:], k.X[t * 128:(t + 1) * 128, :], reads=[k.Xb[t]], writes=[bx])
            transpose_modulate(k, m, x_, bx, v, hT, b_hT, i * 128, tpi)
        if v == 0:
            rc_, rs_, brt = rc[gidx % 2], rs[gidx % 2], b_rt[gidx % 2]
            S.dma("sp", rc_[:, 0:n], k.ropec_in[:, tok0:tok0 + n], writes=[brt])
            S.dma("sp", rs_[:, 0:n], k.ropes_in[:, tok0:tok0 + n], writes=[brt])
        for dst, b_dst, col0, nch in specs:
            for h in range(nch):
                pb = pj % 2
                pj += 1
                for kc in range(8):
                    S.op("pe", lambda e, kc=kc, h=h, pb=pb, col0=col0: e.matmul(k.ps[pb][:, 0:n], lhsT=wX[:, kc, col0 + h * 128:col0 + (h + 1) * 128],
                                                                               rhs=hT[:, kc, 0:n], start=(kc == 0), stop=(kc == 7)),
                         reads=[b_wX, b_hT], writes=[k.psb[pb]], inc=(kc == 7))
                d_ap = dst[:, h, tok0:tok0 + n]
                if v == 0:
                    pq = 2 + pp % 2
                    pp += 1
                    for kc in range(8):
                        S.op("pe", lambda e, kc=kc, h=h, pq=pq, col0=col0: e.matmul(k.ps[pq][:, 0:n], lhsT=wP[:, kc, col0 + h * 128:col0 + (h + 1) * 128],
                                                                                   rhs=hT[:, kc, 0:n], start=(kc == 0), stop=(kc == 7)),
                             reads=[b_wP, b_hT], writes=[k.psb[pq]], inc=(kc == 7))
                    rope_evac(k, k.ps[pb][:, 0:n], k.psb[pb], k.ps[pq][:, 0:n], k.psb[pq], n, d_ap, b_dst, rc_, rs_, brt, W)
                else:
                    S.op("act", lambda e, d_ap=d_ap, pb=pb: e.activation(out=d_ap, in_=k.ps[pb][:, 0:n], func=AF.Copy),
                         reads=[k.psb[pb]], writes=[b_dst])
        vdst, b_vdst, vcol0, nh, dv = vspec
        for i, t in enumerate(tiles if not os.environ.get("ATT_NOV") else []):
            pb = 4 + vv % 2
            vv += 1
            for kc in range(8):
                S.op("pe", lambda e, kc=kc, i=i, pb=pb: e.matmul(k.ps[pb][:, 0:nh * dv], lhsT=hT[:, kc, i * 128:(i + 1) * 128],
                                                                rhs=wX[:, kc, vcol0:vcol0 + nh * dv], start=(kc == 0), stop=(kc == 7)),
                     reads=[b_wX, b_hT], writes=[k.psb[pb]], inc=(kc == 7))
            S.op("dve", lambda e, t=t, pb=pb: e.tensor_copy(out=vdst[:, t, :, 0:dv],
                                                           in_=k.ps[pb][:, 0:nh * dv].rearrange("p (h d) -> p h d", h=nh)),
                 reads=[k.psb[pb]], writes=[b_vdst])


def phase_attn_a(k):
    nc, S = k.nc, k.S
    with ExitStack() as es:
        wA = _sb(k, es, "wA", [128, 8, 1536], BF16)
        b_wA = Buf()
        wsrc = k.attn_w_in[0].rearrange("(c p) n -> p c n", p=128)
        for kc in range(8):
            S.dma("pool", wA[:, kc, :], wsrc[:, kc, 0:1536], writes=[b_wA])
        m = load_mod(k, es, 0, 1, "a")
        QT = _sb(k, es, "QTA", [128, 4, NT * 128], BF16)
        KT = _sb(k, es, "KTA", [128, 4, NT * 128], BF16)
        VA = _sb(k, es, "VA", [128, NT, 4, 132], BF16)
        b_QT, b_KT, b_VA = Buf(), Buf(), Buf()
        S.op("pool", lambda e: e.memset(VA[:].rearrange("p a b c -> p (a b c)"), 1.0), writes=[b_VA])
        if True:
            wPA = _sb(k, es, "wPA", [128, 8, 1024], BF16)
            b_wPA = Buf()
            build_partner_weights(k, wPA, b_wPA, wA, b_wA, 1024)
            attn_inproj(k, es, m, wA, b_wA, wPA, b_wPA, [(QT, b_QT, 0, 4), (KT, b_KT, 512, 4)], (VA, b_VA, 1024, 4, 128), "a")
        if k.stop_after == "attn_ai":
            S.barrier()
            return
        PT = [_sb(k, es, "PT%d" % i, [128, 512], BF16) for i in range(2)]
        b_PT = [Buf(), Buf()]
        AB = [_sb(k, es, "ABa%d" % i, [128, 512], F32) for i in range(4)]
        b_AB = [Buf() for _ in range(4)]
        ta = [_sb(k, es, "ta%d" % i, [128, 128], F32) for i in range(2)]
        junk = [_sb(k, es, "junk%d" % i, [128, 128], F32) for i in range(2)]
        sm = [_sb(k, es, "asm%d" % i, [128, 8], F32) for i in range(2)]
        b_ta, b_junk, b_sm = [Buf(), Buf()], [Buf(), Buf()], [Buf(), Buf()]
        qgroups = [(g * 256, list(range(NT))) for g in range(T // 256)] + [(T, [NTX, NTX + 1])]
        si = fi = 0
        for qg, (q0, ktiles) in enumerate(qgroups):
            for h in range(4):
                for ki, kt in enumerate(ktiles):
                    pbs = (0, 1) if si % 2 == 0 else (6, 7)
                    PT_ = PT[si % 2]
                    bpt = b_PT[si % 2]
                    si += 1
                    for sub in range(2):
                        S.op("pe", lambda e, sub=sub, h=h, kt=kt, pbs=pbs: e.matmul(
                            k.ps[pbs[sub]][:, 0:256], lhsT=KT[sub * 64:(sub + 1) * 64, h, kt * 128:(kt + 1) * 128],
                            rhs=QT[sub * 64:(sub + 1) * 64, h, q0:q0 + 256], start=True, stop=True),
                             reads=[b_KT, b_QT], writes=[k.psb[pbs[sub]]])
                    for sub in range(2):
                        S.op("act", lambda e, sub=sub, pbs=pbs, PT_=PT_: e.activation(out=PT_[:, sub * 256:(sub + 1) * 256], in_=k.ps[pbs[sub]][:, 0:256],
                                                                                  func=AF.Exp, scale=0.125),
                             reads=[k.psb[pbs[sub]]], writes=[bpt])
                    last = ki == len(ktiles) - 1
                    for sub in range(2):
                        for qt in range(2):
                            ab = 2 + sub * 2 + qt
                            S.op("pe", lambda e, sub=sub, qt=qt, ab=ab, PT_=PT_, kt=kt, h=h, ki=ki, last=last: e.matmul(
                                k.ps[ab][:, 0:129], lhsT=PT_[:, sub * 256 + qt * 128:sub * 256 + (qt + 1) * 128],
                                rhs=VA[:, kt, h, 0:129], start=(ki == 0), stop=last),
                                 reads=[bpt, b_VA], writes=[k.psb[ab]], inc=(last and sub == 1 and qt == 1))
                for qt in range(2):
                    b1, b2 = 2 + qt, 4 + qt
                    s_ = sm[fi % 2]
                    bs = b_sm[fi % 2]
                    t_ = ta[fi % 2]
                    bt = b_ta[fi % 2]
                    j_ = junk[fi % 2]
                    bj = b_junk[fi % 2]
                    fi += 1
                    ab_ = AB[(qg % 2) * 2 + qt]
                    bab = b_AB[(qg % 2) * 2 + qt]
                    S.op("dve", lambda e, s_=s_, b1=b1: e.reciprocal(out=s_[:, 0:1], in_=k.ps[b1][:, 128:129]), reads=[k.psb[b1]], writes=[bs])
                    S.op("dve", lambda e, s_=s_, b2=b2: e.reciprocal(out=s_[:, 1:2], in_=k.ps[b2][:, 128:129]), reads=[k.psb[b2]], writes=[bs])
                    S.op("dve", lambda e, s_=s_: e.tensor_scalar(out=s_[:, 2:3], in0=s_[:, 1:2], scalar1=k.neglam, scalar2=None, op0=ALU.mult),
                         reads=[bs, k.b_cst], writes=[bs])
                    S.op("dve", lambda e, s_=s_, t_=t_, b1=b1: e.tensor_scalar(out=t_[:], in0=k.ps[b1][:, 0:128], scalar1=s_[:, 0:1], scalar2=None,
                                                                              op0=ALU.mult), reads=[k.psb[b1], bs], writes=[bt])
                    S.op("dve", lambda e, s_=s_, t_=t_, b2=b2: e.scalar_tensor_tensor(out=t_[:], in0=k.ps[b2][:, 0:128], scalar=s_[:, 2:3], in1=t_[:],
                                                                                     op0=ALU.mult, op1=ALU.add),
                         reads=[k.psb[b2], bs, bt], writes=[bt])
                    S.op("act", lambda e, s_=s_, t_=t_, j_=j_: e.activation(out=j_[:], in_=t_[:], func=AF.Square, accum_out=s_[:, 3:4]),
                         reads=[bt], writes=[bj, bs])
                    S.op("act", lambda e, s_=s_: e.activation(out=s_[:, 4:5], in_=s_[:, 3:4], func=AF.Sqrt, scale=1.0 / 128.0, bias=float(EPS)),
                         reads=[bs], writes=[bs])
                    S.op("dve", lambda e, s_=s_: e.reciprocal(out=s_[:, 5:6], in_=s_[:, 4:5]), reads=[bs], writes=[bs])
                    S.op("dve", lambda e, s_=s_, t_=t_, ab_=ab_, h=h: e.scalar_tensor_tensor(out=ab_[:, h * 128:(h + 1) * 128], in0=t_[:], scalar=s_[:, 5:6],
                                                                                           in1=k.g08[:], op0=ALU.mult, op1=ALU.mult),
                         reads=[bt, bs, k.b_cst], writes=[bab])
            for qt in range(2):
                t = q0 // 128 + qt
                S.dma("sp", k.ABd[t * 128:(t + 1) * 128, :], AB[(qg % 2) * 2 + qt][:], reads=[b_AB[(qg % 2) * 2 + qt]], writes=[k.ABb[t]])
        S.barrier()


def phase_attn_b(k):
    nc, S = k.nc, k.S
    with ExitStack() as es:
        wB = _sb(k, es, "wB", [128, 8, 768], BF16)
        wo = _sb(k, es, "wo", [128, 8, D], BF16)
        b_wB, b_wo = Buf(), Buf()
        wsrc = k.attn_w_in[0].rearrange("(c p) n -> p c n", p=128)
        for kc in range(8):
            for hf in range(2):
                S.dma("pool", wB[:, kc, 0:512].rearrange("p (g hf d) -> p g hf d", g=4, hf=2)[:, :, hf, :],
                      wsrc[:, kc, 1536:2048].rearrange("p (hf g d) -> p hf g d", g=4, hf=2)[:, hf, :, :], writes=[b_wB])
            S.dma("pool", wB[:, kc, 512:768], wsrc[:, kc, 2048:2304], writes=[b_wB])
        wosrc = k.attn_w_out[0].rearrange("(c p) n -> p c n", p=128)
        for kc in range(8):
            S.dma("pool", wo[:, kc, :], wosrc[:, kc, :], writes=[b_wo])
        m = load_mod(k, es, 0, 1, "b")
        QT = _sb(k, es, "QTB", [128, 4, NT * 128], BF16)
        KT = _sb(k, es, "KTB", [128, 1, NT * 128], BF16)
        VB = _sb(k, es, "VB", [128, NT, 2, 66], BF16)
        b_QT, b_KT, b_VB = Buf(), Buf(), Buf()
        S.op("pool", lambda e: e.memset(VB[:].rearrange("p a b c -> p (a b c)"), 1.0), writes=[b_VB])
        if True:
            wPB = _sb(k, es, "wPB", [128, 8, 640], BF16)
            b_wPB = Buf()
            build_partner_weights(k, wPB, b_wPB, wB, b_wB, 640)
            attn_inproj(k, es, m, wB, b_wB, wPB, b_wPB, [(QT, b_QT, 0, 4), (KT, b_KT, 512, 1)], (VB, b_VB, 640, 2, 64), "b")
        PT = [_sb(k, es, "PTb%d" % i, [128, 512], BF16) for i in range(2)]
        b_PT = [Buf(), Buf()]
        AB = [_sb(k, es, "ABb%d" % i, [128, D], F32) for i in range(2)]
        b_AB = [Buf(), Buf()]
        abT = _sb(k, es, "abT", [128, 8, 128], BF16)
        b_abT = Buf()
        xr = [_sb(k, es, "xrb%d" % i, [128, D], F32) for i in range(2)]
        ot = [_sb(k, es, "otb%d" % i, [128, D], F32) for i in range(2)]
        small = [_sb(k, es, "smb%d" % i, [128, 32], F32) for i in range(2)]
        sm = [_sb(k, es, "bsm%d" % i, [128, 8], F32) for i in range(2)]
        b_xr, b_ot, b_small, b_sm = ([Buf(), Buf()] for _ in range(4))
        si = fi = 0
        tpi = [0]
        for qt in range(NT):
            v = 0 if qt < NTX else 1
            if v == 0:
                kts = ([(qt - 1, k.maskp)] if qt > 0 else []) + [(qt, None)] + ([(qt + 1, k.maskn)] if qt < NTX - 1 else [])
                kts += [(NTX, None), (NTX + 1, None)]
            else:
                kts = [(NTX, None), (NTX + 1, None)]
            abt = AB[qt % 2]
            bab = b_AB[qt % 2]
            S.dma("sp", abt[:, 0:512], k.ABd[qt * 128:(qt + 1) * 128, :], reads=[k.ABb[qt]], writes=[bab])
            for kvh in range(2):
                p0, p1 = kvh * 64, (kvh + 1) * 64
                for ki, (kt, mk) in enumerate(kts):
                    pb = si % 2
                    PT_ = PT[si % 2]
                    bpt = b_PT[si % 2]
                    si += 1
                    S.op("pe", lambda e, kt=kt, pb=pb, p0=p0, p1=p1: e.matmul(
                        k.ps[pb][:, :].rearrange("p (g q) -> p g q", g=4), lhsT=KT[p0:p1, 0, kt * 128:(kt + 1) * 128],
                        rhs=QT[p0:p1, :, qt * 128:(qt + 1) * 128], start=True, stop=True),
                         reads=[b_KT, b_QT], writes=[k.psb[pb]])
                    S.op("act", lambda e, pb=pb, PT_=PT_: e.activation(out=PT_[:], in_=k.ps[pb][:, :], func=AF.Exp, scale=0.125),
                         reads=[k.psb[pb]], writes=[bpt])
                    if mk is not None:
                        S.op("pool", lambda e, PT_=PT_, mk=mk: e.tensor_tensor(out=PT_[:], in0=PT_[:], in1=mk[:], op=ALU.mult),
                             reads=[bpt, k.b_cst], writes=[bpt])
                    last = ki == len(kts) - 1
                    for g in range(4):
                        S.op("pe", lambda e, g=g, PT_=PT_, kt=kt, kvh=kvh, ki=ki, last=last: e.matmul(
                            k.ps[2 + g][:, 0:65], lhsT=PT_[:, g * 128:(g + 1) * 128], rhs=VB[:, kt, kvh, 0:65], start=(ki == 0), stop=last),
                             reads=[bpt, b_VB], writes=[k.psb[2 + g]], inc=(last and g == 3))
                for g in range(4):
                    head = kvh * 4 + g
                    s_ = sm[fi % 2]
                    bs = b_sm[fi % 2]
                    fi += 1
                    S.op("dve", lambda e, s_=s_, g=g, head=head: e.tensor_tensor(out=s_[:, 0:1], in0=k.ps[2 + g][:, 64:65],
                                                                                in1=k.esink[:, head:head + 1], op=ALU.add),
                         reads=[k.psb[2 + g], k.b_cst], writes=[bs])
                    S.op("dve", lambda e, s_=s_: e.reciprocal(out=s_[:, 1:2], in_=s_[:, 0:1]), reads=[bs], writes=[bs])
                    S.op("dve", lambda e, s_=s_, g=g, head=head, abt=abt: e.tensor_scalar(out=abt[:, 512 + head * 64:512 + (head + 1) * 64],
                                                                                        in0=k.ps[2 + g][:, 0:64], scalar1=s_[:, 1:2], scalar2=None,
                                                                                        op0=ALU.mult),
                         reads=[k.psb[2 + g], bs], writes=[bab])
            if k.dbg_on:
                S.dma("sp", k.BBd[qt * 128:(qt + 1) * 128, :], abt[:, 512:1024], reads=[bab], writes=[k.BBb[qt]])
            transpose_modulate(k, None, abt, bab, v, abT, b_abT, 0, tpi, banks=(6,))
            xr_, bxr = xr[qt % 2], b_xr[qt % 2]
            ot_, bot = ot[qt % 2], b_ot[qt % 2]
            S.dma("sp", xr_[:], k.X[qt * 128:(qt + 1) * 128, :], reads=[k.Xb[qt]], writes=[bxr])
            for h2 in range(2):
                for fc in range(8):
                    S.op("pe", lambda e, fc=fc, h2=h2: e.matmul(k.ps[7][:, :], lhsT=abT[:, fc, :], rhs=wo[:, fc, h2 * 512:(h2 + 1) * 512],
                                                               start=(fc == 0), stop=(fc == 7)),
                         reads=[b_abT, b_wo], writes=[k.psb[7]], inc=(fc == 7))
                pn_part(k, m, v, 1.0, h2, k.ps[7][:, :], k.psb[7], ot_, bot)
            pn_finish(k, m, xr_, bxr, ot_, bot, small[qt % 2], b_small[qt % 2], k.X[qt * 128:(qt + 1) * 128, :], [k.Xb[qt]])
        S.barrier()


RC = dict(mq=0, mk=512, mv=1024, mo=1536, mg=2048, rq=2064, rk=2320, rv=2576, rg=3088)


def load_rec_consts(k, es):
    S = k.S
    c = K()
    k.rc = c
    c.b = Buf()
    f32 = lambda name, shape: _sb(k, es, name, shape, F32)
    c.tri = [f32("tri%d" % d, [128, 128]) for d in range(2)]
    c.mb = [f32("mb%d" % d, [128, 128]) for d in range(2)]
    c.mw = [f32("mw%d" % d, [128, 128]) for d in range(2)]
    c.mall = f32("mall", [128, 128])
    c.pos = f32("posc", [128, 8])
    c.gb = f32("gateb", [128, 16])
    c.lg = f32("lgam", [128, 8])
    c.rcol = f32("rcol", [128, 32])
    c.mm = [f32("mm%d" % d, [128, 128]) for d in range(2)]
    c.dj = [[f32("dj%d%d" % (d, h), [128, 128]) for h in range(4)] for d in range(2)]
    c.gg = f32("ggrow", [128, D])
    c.rb = f32("rbrow", [128, 512])
    for d in range(2):
        S.dma("sp", c.tri[d][:], k.tri_in[d], writes=[c.b])
        S.dma("sp", c.mb[d][:], k.mb_in[d], writes=[c.b])
        S.dma("sp", c.mw[d][:], k.mw_in[d], writes=[c.b])
    S.dma("sp", c.mall[:], k.mall_in[:, :], writes=[c.b])
    S.dma("sp", c.pos[:], k.pos_in[:, :], writes=[c.b])
    S.dma("sp", c.gb[:], k.mlstm_gate_b[0].rearrange("a b c -> (a b c)").partition_broadcast(128), writes=[c.b])
    S.dma("sp", c.lg[:], k.ret_decay_logit[0].rearrange("a b -> (a b)").partition_broadcast(128), writes=[c.b])
    S.dma("sp", c.gg[:, 0:512], k.mlstm_norm_g[0, :].partition_broadcast(128), writes=[c.b])
    S.dma("sp", c.gg[:, 512:1024], k.ret_norm_g[0, :].partition_broadcast(128), writes=[c.b])
    S.dma("sp", c.rb[:], k.ret_norm_b[0, :].partition_broadcast(128), writes=[c.b])
    rw = [c.b]
    S.op("act", lambda e: e.activation(out=c.lg[:], in_=c.lg[:], func=AF.Exp, scale=-1.0), reads=rw, writes=rw)
    S.op("act", lambda e: e.activation(out=c.lg[:], in_=c.lg[:], func=AF.Ln, bias=1.0, scale=1.0), reads=rw, writes=rw)
    S.op("dve", lambda e: e.tensor_scalar_mul(out=c.lg[:], in0=c.lg[:], scalar1=-1.0), reads=rw, writes=rw)
    for d in range(2):
        S.op("act", lambda e, d=d: e.activation(out=c.rcol[:, d * 4:d * 4 + 4], in_=c.lg[:, d * 4:d * 4 + 4], func=AF.Exp,
                                                scale=c.pos[:, d:d + 1]), reads=rw, writes=rw)
        S.op("act", lambda e, d=d: e.activation(out=c.rcol[:, 8 + d * 4:8 + d * 4 + 4], in_=c.lg[:, d * 4:d * 4 + 4], func=AF.Exp,
                                                scale=c.pos[:, 2 + d:3 + d]), reads=rw, writes=rw)
        S.op("act", lambda e, d=d: e.activation(out=c.rcol[:, 16 + d * 4:16 + d * 4 + 4], in_=c.lg[:, d * 4:d * 4 + 4], func=AF.Exp,
                                                scale=c.pos[:, 4 + d:5 + d]), reads=rw, writes=rw)
    S.op("act", lambda e: e.activation(out=c.rcol[:, 24:32], in_=c.lg[:], func=AF.Exp, scale=128.0), reads=rw, writes=rw)
    for d in range(2):
        S.op("dve", lambda e, d=d: e.tensor_scalar_mul(out=c.mm[d][:], in0=c.tri[d][:], scalar1=float(128.0 ** -0.5)), reads=rw, writes=rw)
        for h in range(4):
            S.op("dve", lambda e, d=d, h=h: e.tensor_scalar(out=c.dj[d][h][:], in0=c.tri[d][:], scalar1=c.rcol[:, d * 4 + h:d * 4 + h + 1],
                                                           scalar2=0.125, op0=ALU.mult, op1=ALU.mult), reads=rw, writes=rw)
    c.glp = f32("glp", [128, 4])
    for d in range(2):
        for p in range(2):
            for hf in range(2):
                r0 = hf * 64
                src = 24 + d * 4 + 2 * p + hf
                S.op("dve", lambda e, d=d, p=p, r0=r0, src=src: e.tensor_copy(out=c.glp[r0:r0 + 64, d * 2 + p:d * 2 + p + 1],
                                                                             in_=c.rcol[r0:r0 + 64, src:src + 1]), reads=rw, writes=rw)


def phase_rec(k, d):
    nc, S = k.nc, k.S
    c = k.rc
    l = 1
    with ExitStack() as es:
        wR = _sb(k, es, "wR", [128, 8, REC_IN], BF16)
        b_wR = Buf()
        wsrc = k.rec_w_in[0].rearrange("(c p) n -> p c n", p=128)
        for kc in range(8):
            for c0 in (0, 1800):
                S.dma("pool", wR[:, kc, c0:c0 + 1800], wsrc[:, kc, c0:c0 + 1800], writes=[b_wR])
        if d == 1:
            wo = _sb(k, es, "wor", [128, 8, D], BF16)
            b_wo = Buf()
            wosrc = k.rec_w_out[0].rearrange("(c p) n -> p c n", p=128)
            for kc in range(8):
                S.dma("pool", wo[:, kc, :], wosrc[:, kc, :], writes=[b_wo])
        m = load_mod(k, es, l, 1, "r%d" % d)
        bf = lambda name, shape: _sb(k, es, name + "_%d" % d, shape, BF16)
        f32 = lambda name, shape: _sb(k, es, name + "_%d" % d, shape, F32)
        xt = [f32("rxt%d" % i, [128, D]) for i in range(2)]
        b_xt = [Buf(), Buf()]
        hT = bf("rhT", [128, 8, 128])
        b_hT = Buf()
        FT = bf("FT", [128, 12, 128])
        b_FT = Buf()
        VA = bf("rVA", [128, 4, 132])
        RV = bf("rRV", [128, 4, 128])
        b_VA, b_RV = Buf(), Buf()
        S.op("pool", lambda e: e.memset(VA[:].rearrange("p a b -> p (a b)"), 1.0), writes=[b_VA])
        G = f32("rG", [128, 8])
        LF = f32("rLF", [128, 4])
        PK = f32("rPK", [128, 16])
        E = f32("rE", [128, 16])
        b_G, b_LF, b_PK, b_E = Buf(), Buf(), Buf(), Buf()
        Sb = [bf("rSb%d" % i, [128, 128]) for i in range(2)]
        b_Sb = [Buf(), Buf()]
        Kpp = [bf("rKpp%d" % i, [128, 128]) for i in range(2)]
        b_Kpp = [Buf(), Buf()]
        Kpad = [[bf("rKpad%d%d" % (p, hf), [128, 128]) for hf in range(2)] for p in range(2)]
        b_Kpad = [[Buf(), Buf()], [Buf(), Buf()]]
        for p in range(2):
            for hf in range(2):
                S.op("pool", lambda e, p=p, hf=hf: e.memset(Kpad[p][hf][:], 0.0), writes=[b_Kpad[p][hf]])
        C = [f32("rC%d" % h, [128, 132]) for h in range(4)]
        Cb = [bf("rCb%d" % h, [128, 132]) for h in range(4)]
        b_C, b_Cb = [Buf() for _ in range(4)], [Buf() for _ in range(4)]
        Sp = [f32("rSp%d" % p, [128, 128]) for p in range(2)]
        Spb = [bf("rSpb%d" % p, [128, 128]) for p in range(2)]
        b_Sp, b_Spb = [Buf(), Buf()], [Buf(), Buf()]
        for h in range(4):
            S.op("pool", lambda e, h=h: e.memset(C[h][:], 0.0), writes=[b_C[h]])
            S.op("pool", lambda e, h=h: e.memset(Cb[h][:], 0.0), writes=[b_Cb[h]])
        for p in range(2):
            S.op("pool", lambda e, p=p: e.memset(Sp[p][:], 0.0), writes=[b_Sp[p]])
            S.op("pool", lambda e, p=p: e.memset(Spb[p][:], 0.0), writes=[b_Spb[p]])
        H = [f32("rH%d" % i, [128, D]) for i in range(2)]
        b_H = [Buf(), Buf()]
        fs = [f32("rfs%d" % i, [128, 8]) for i in range(2)]
        b_fs = [Buf(), Buf()]
        if d == 1:
            H0 = [f32("rH0%d" % i, [128, D]) for i in range(2)]
            b_H0 = [Buf(), Buf()]
            gsig = f32("rgs", [128, D])
            b_gsig = Buf()
            stt = f32("rstt", [128, 8, 6])
            mv8 = f32("rmv8", [128, 8, 2])
            sc8 = f32("rsc8", [128, 24])
            b_st = Buf()
            zT = bf("rzT", [128, 8, 128])
            b_zT = Buf()
            xr = [f32("rxr%d" % i, [128, D]) for i in range(2)]
            ot = [f32("rot%d" % i, [128, D]) for i in range(2)]
            small = [f32("rsm%d" % i, [128, 32]) for i in range(2)]
            b_xr, b_ot, b_small = ([Buf(), Buf()] for _ in range(3))
        order = [NTX, NTX + 1] + list(range(NTX)) if d == 0 else [NTX + 1, NTX] + list(range(NTX - 1, -1, -1))
        tpi = [0]
        si = ki = fi = 0
        for ci, t in enumerate(order):
            v = 0 if t < NTX else 1
            need_out = v == 0
            x_, bx = xt[ci % 2], b_xt[ci % 2]
            S.dma("sp", x_[:], k.X[t * 128:(t + 1) * 128, :], reads=[k.Xb[t]], writes=[bx])
            transpose_modulate(k, m, x_, bx, v, hT, b_hT, 0, tpi, banks=(6,))
            fcols = [RC["mq"] + h * 128 for h in range(4)] + [RC["mk"] + h * 128 for h in range(4)] + \
                    [RC["rq"], RC["rq"] + 128, RC["rk"], RC["rk"] + 128]
            for grp in range(3):
                pb = grp % 2
                for j4 in range(4):
                    col = fcols[grp * 4 + j4]
                    for kc in range(8):
                        S.op("pe", lambda e, kc=kc, col=col, j4=j4, pb=pb: e.matmul(k.ps[pb][:, j4 * 128:(j4 + 1) * 128], lhsT=wR[:, kc, col:col + 128],
                                                                                  rhs=hT[:, kc, :], start=(kc == 0), stop=(kc == 7)),
                             reads=[b_wR, b_hT], writes=[k.psb[pb]], inc=(kc == 7 and j4 == 3))
                S.op("act", lambda e, grp=grp, pb=pb: e.activation(out=FT[:, grp * 4:(grp + 1) * 4, :].rearrange("p a b -> p (a b)"), in_=k.ps[pb][:, :],
                                                                  func=AF.Copy), reads=[k.psb[pb]], writes=[b_FT])
            def tm(pb, col, n, c0=0, last=True):
                for kc in range(8):
                    S.op("pe", lambda e, kc=kc: e.matmul(k.ps[pb][:, c0:c0 + n], lhsT=hT[:, kc, :], rhs=wR[:, kc, col:col + n],
                                                         start=(kc == 0), stop=(kc == 7)),
                         reads=[b_wR, b_hT], writes=[k.psb[pb]], inc=(kc == 7))
            tm(2, RC["mk"], 512)
            tm(3, RC["mv"], 512)
            tm(4, RC["rk"], 256)
            tm(4, RC["mg"], 16, c0=256)
            tm(5, RC["rv"], 512)
            S.op("dve", lambda e: e.tensor_copy(out=VA[:, :, 0:128], in_=k.ps[3][:, :].rearrange("p (h d) -> p h d", h=4)),
                 reads=[k.psb[3]], writes=[b_VA])
            S.op("act", lambda e: e.activation(out=RV[:].rearrange("p a b -> p (a b)"), in_=k.ps[5][:, :], func=AF.Copy),
                 reads=[k.psb[5]], writes=[b_RV])
            S.op("dve", lambda e: e.tensor_tensor(out=G[:], in0=k.ps[4][:, 256 + d * 8:256 + d * 8 + 8], in1=c.gb[:, d * 8:d * 8 + 8], op=ALU.add),
                 reads=[k.psb[4], c.b], writes=[b_G])
            S.op("act", lambda e: e.activation(out=LF[:], in_=G[:, 4:8], func=AF.Exp, scale=-1.0), reads=[b_G], writes=[b_LF])
            S.op("act", lambda e: e.activation(out=LF[:], in_=LF[:], func=AF.Ln, bias=1.0, scale=1.0), reads=[b_LF], writes=[b_LF])
            for ji, mat in enumerate((c.mb[d], c.mw[d], c.mall)):
                S.op("pe", lambda e, ji=ji, mat=mat: e.matmul(k.ps[7][:, ji * 4:ji * 4 + 4], lhsT=mat[:], rhs=LF[:], start=True, stop=True),
                     reads=[c.b, b_LF], writes=[k.psb[7]], inc=(ji == 2))
            S.op("dve", lambda e: e.tensor_tensor(out=PK[:, 0:4], in0=G[:, 0:4], in1=k.ps[7][:, 0:4], op=ALU.subtract),
                 reads=[b_G, k.psb[7]], writes=[b_PK])
            S.op("dve", lambda e: e.tensor_tensor(out=PK[:, 4:8], in0=G[:, 0:4], in1=k.ps[7][:, 4:8], op=ALU.add),
                 reads=[b_G, k.psb[7]], writes=[b_PK])
            S.op("dve", lambda e: e.tensor_copy(out=PK[:, 8:12], in_=k.ps[7][:, 0:4]), reads=[k.psb[7]], writes=[b_PK])
            S.op("dve", lambda e: e.tensor_copy(out=PK[:, 12:16], in_=k.ps[7][:, 8:12]), reads=[k.psb[7]], writes=[b_PK])
            S.op("act", lambda e: e.activation(out=E[:], in_=PK[:], func=AF.Exp), reads=[b_PK], writes=[b_E])
            H_, bH = H[ci % 2], b_H[ci % 2]
            for h in range(4):
                if need_out:
                    sb_, bsb = Sb[si % 2], b_Sb[si % 2]
                    si += 1
                    S.op("pe", lambda e, h=h: e.matmul(k.ps[0][:, 0:128], lhsT=FT[:, 4 + h, :], rhs=FT[:, h, :], start=True, stop=True),
                         reads=[b_FT], writes=[k.psb[0]])
                    S.op("dve", lambda e, h=h, sb_=sb_: e.scalar_tensor_tensor(out=sb_[:], in0=k.ps[0][:, 0:128], scalar=E[:, h:h + 1], in1=c.mm[d][:],
                                                                              op0=ALU.mult, op1=ALU.mult),
                         reads=[k.psb[0], b_E, c.b], writes=[bsb])
                    S.op("pe", lambda e, h=h, sb_=sb_: e.matmul(k.ps[1][:, 0:129], lhsT=sb_[:], rhs=VA[:, h, 0:129], start=True, stop=False),
                         reads=[bsb, b_VA], writes=[k.psb[1]], inc=False)
                    S.op("pe", lambda e, h=h: e.matmul(k.ps[1][:, 0:129], lhsT=FT[:, h, :], rhs=Cb[h][:, 0:129], start=False, stop=True),
                         reads=[b_FT, b_Cb[h]], writes=[k.psb[1]])
                    f_, bf_ = fs[fi % 2], b_fs[fi % 2]
                    fi += 1
                    S.op("dve", lambda e, h=h, f_=f_: e.tensor_tensor(out=f_[:, 0:1], in0=k.ps[1][:, 128:129], in1=E[:, 8 + h:9 + h], op=ALU.mult),
                         reads=[k.psb[1], b_E], writes=[bf_])
                    S.op("dve", lambda e, f_=f_: e.tensor_scalar_mul(out=f_[:, 4:5], in0=f_[:, 0:1], scalar1=-1.0), reads=[bf_], writes=[bf_])
                    S.op("dve", lambda e, f_=f_: e.tensor_tensor(out=f_[:, 1:2], in0=f_[:, 0:1], in1=f_[:, 4:5], op=ALU.max), reads=[bf_], writes=[bf_])
                    S.op("dve", lambda e, f_=f_: e.tensor_scalar_max(out=f_[:, 1:2], in0=f_[:, 1:2], scalar1=1.0), reads=[bf_], writes=[bf_])
                    S.op("dve", lambda e, f_=f_: e.reciprocal(out=f_[:, 2:3], in_=f_[:, 1:2]), reads=[bf_], writes=[bf_])
                    S.op("dve", lambda e, h=h, f_=f_: e.tensor_tensor(out=f_[:, 3:4], in0=f_[:, 2:3], in1=E[:, 8 + h:9 + h], op=ALU.mult),
                         reads=[bf_, b_E], writes=[bf_])
                    S.op("dve", lambda e, h=h, f_=f_, H_=H_: e.tensor_scalar(out=H_[:, h * 128:(h + 1) * 128], in0=k.ps[1][:, 0:128], scalar1=f_[:, 3:4],
                                                                           scalar2=None, op0=ALU.mult),
                         reads=[k.psb[1], bf_], writes=[bH])
                kp_, bkp = Kpp[ki % 2], b_Kpp[ki % 2]
                ki += 1
                S.op("act" if h % 2 == 0 else "dve",
                     (lambda e, h=h, kp_=kp_: e.activation(out=kp_[:], in_=k.ps[2][:, h * 128:(h + 1) * 128], func=AF.Identity, scale=E[:, 4 + h:5 + h]))
                     if h % 2 == 0 else
                     (lambda e, h=h, kp_=kp_: e.tensor_scalar(out=kp_[:], in0=k.ps[2][:, h * 128:(h + 1) * 128], scalar1=E[:, 4 + h:5 + h], scalar2=None,
                                                            op0=ALU.mult)),
                     reads=[k.psb[2], b_E], writes=[bkp])
                S.op("pe", lambda e, h=h, kp_=kp_: e.matmul(k.ps[6][:, 0:129], lhsT=kp_[:], rhs=VA[:, h, 0:129], start=True, stop=True),
                     reads=[bkp, b_VA], writes=[k.psb[6]])
                S.op("dve", lambda e, h=h: e.tensor_scalar(out=C[h][:, 0:129], in0=C[h][:, 0:129], scalar1=E[:, 12 + h:13 + h], scalar2=None, op0=ALU.mult),
                     reads=[b_C[h], b_E], writes=[b_C[h]])
                S.op("dve", lambda e, h=h: e.scalar_tensor_tensor(out=C[h][:, 0:129], in0=k.ps[6][:, 0:129], scalar=float(128.0 ** -0.5), in1=C[h][:, 0:129],
                                                                 op0=ALU.mult, op1=ALU.add),
                     reads=[k.psb[6], b_C[h]], writes=[b_C[h]])
                S.op("act", lambda e, h=h: e.activation(out=Cb[h][:, 0:129], in_=C[h][:, 0:129], func=AF.Copy), reads=[b_C[h]], writes=[b_Cb[h]])
            for h in range(4):
                p, hf = h // 2, h % 2
                r0 = hf * 64
                if need_out:
                    sb_, bsb = Sb[si % 2], b_Sb[si % 2]
                    si += 1
                    S.op("pe", lambda e, p=p, r0=r0: e.matmul(k.ps[0][:, 0:128], lhsT=FT[r0:r0 + 64, 10 + p, :], rhs=FT[r0:r0 + 64, 8 + p, :],
                                                              start=True, stop=True), reads=[b_FT], writes=[k.psb[0]])
                    S.op("dve", lambda e, h=h, sb_=sb_: e.tensor_tensor(out=sb_[:], in0=k.ps[0][:, 0:128], in1=c.dj[d][h][:], op=ALU.mult),
                         reads=[k.psb[0], c.b], writes=[bsb])
                    S.op("pe", lambda e, h=h, sb_=sb_: e.matmul(k.ps[1][:, 0:128], lhsT=sb_[:], rhs=RV[:, h, :], start=True, stop=False),
                         reads=[bsb, b_RV], writes=[k.psb[1]], inc=False)
                    S.op("pe", lambda e, p=p, r0=r0: e.matmul(k.ps[1][:, 0:128], lhsT=FT[r0:r0 + 64, 8 + p, :], rhs=Spb[p][r0:r0 + 64, :],
                                                              start=False, stop=True), reads=[b_FT, b_Spb[p]], writes=[k.psb[1]])
                    S.op("act", lambda e, h=h, H_=H_: e.activation(out=H_[:, 512 + h * 128:512 + (h + 1) * 128], in_=k.ps[1][:, 0:128], func=AF.Identity,
                                                                  scale=c.rcol[:, 8 + d * 4 + h:8 + d * 4 + h + 1]),
                         reads=[k.psb[1], c.b], writes=[bH])
                S.op("dve", lambda e, h=h, p=p, hf=hf, r0=r0: e.tensor_scalar(out=Kpad[p][hf][:, r0:r0 + 64], in0=k.ps[4][:, h * 64:(h + 1) * 64],
                                                                            scalar1=c.rcol[:, 16 + d * 4 + h:16 + d * 4 + h + 1], scalar2=0.125,
                                                                            op0=ALU.mult, op1=ALU.mult),
                     reads=[k.psb[4], c.b], writes=[b_Kpad[p][hf]])
                if hf == 1:
                    S.op("pe", lambda e, p=p: e.matmul(k.ps[6][:, 0:128], lhsT=Kpad[p][0][:], rhs=RV[:, 2 * p, :], start=True, stop=False),
                         reads=[b_Kpad[p][0], b_RV], writes=[k.psb[6]], inc=False)
                    S.op("pe", lambda e, p=p: e.matmul(k.ps[6][:, 0:128], lhsT=Kpad[p][1][:], rhs=RV[:, 2 * p + 1, :], start=False, stop=True),
                         reads=[b_Kpad[p][1], b_RV], writes=[k.psb[6]])
                    S.op("dve", lambda e, p=p: e.scalar_tensor_tensor(out=Sp[p][:], in0=Sp[p][:], scalar=c.glp[:, d * 2 + p:d * 2 + p + 1], in1=k.ps[6][:, 0:128],
                                                                     op0=ALU.mult, op1=ALU.add),
                         reads=[b_Sp[p], c.b, k.psb[6]], writes=[b_Sp[p]])
                    S.op("act", lambda e, p=p: e.activation(out=Spb[p][:], in_=Sp[p][:], func=AF.Copy), reads=[b_Sp[p]], writes=[b_Spb[p]])
            if not need_out:
                continue
            if d == 0:
                S.dma("sp", k.H0d[t * 128:(t + 1) * 128, :], H_[:], reads=[bH], writes=[k.H0b[t]])
                continue
            tm(2, RC["mo"], 512)
            tm(3, RC["rg"], 512)
            S.op("act", lambda e: e.activation(out=gsig[:, 0:512], in_=k.ps[2][:, :], func=AF.Sigmoid), reads=[k.psb[2]], writes=[b_gsig])
            S.op("act", lambda e: e.activation(out=gsig[:, 512:1024], in_=k.ps[3][:, :], func=AF.Silu), reads=[k.psb[3]], writes=[b_gsig])
            h0_, bh0 = H0[ci % 2], b_H0[ci % 2]
            S.dma("sp", h0_[:], k.H0d[t * 128:(t + 1) * 128, :], reads=[k.H0b[t]], writes=[bh0])
            S.op("pool", lambda e, H_=H_, h0_=h0_: e.tensor_tensor(out=H_[:], in0=H_[:], in1=h0_[:], op=ALU.add), reads=[bH, bh0], writes=[bH])
            for blk in range(8):
                S.op("dve", lambda e, blk=blk, H_=H_: e.bn_stats(out=stt[:, blk, :], in_=H_[:, blk * 128:(blk + 1) * 128]), reads=[bH], writes=[b_st])
            for blk in range(8):
                S.op("dve", lambda e, blk=blk: e.bn_aggr(out=mv8[:, blk, :], in_=stt[:, blk, :]), reads=[b_st], writes=[b_st])
            S.op("act", lambda e: e.activation(out=sc8[:, 0:8], in_=mv8[:, :, 1], func=AF.Sqrt, bias=float(EPS), scale=1.0), reads=[b_st], writes=[b_st])
            S.op("dve", lambda e: e.reciprocal(out=sc8[:, 8:16], in_=sc8[:, 0:8]), reads=[b_st], writes=[b_st])
            S.op("dve", lambda e: e.scalar_tensor_tensor(out=sc8[:, 16:24], in0=mv8[:, :, 0], scalar=-1.0, in1=sc8[:, 8:16], op0=ALU.mult, op1=ALU.mult),
                 reads=[b_st], writes=[b_st])
            for blk in range(8):
                S.op("act", lambda e, blk=blk, H_=H_: e.activation(out=H_[:, blk * 128:(blk + 1) * 128], in_=H_[:, blk * 128:(blk + 1) * 128], func=AF.Identity,
                                                                  bias=sc8[:, 16 + blk:17 + blk], scale=sc8[:, 8 + blk:9 + blk]),
                     reads=[bH, b_st], writes=[bH])
            S.op("pool", lambda e, H_=H_: e.tensor_tensor(out=H_[:], in0=H_[:], in1=c.gg[:], op=ALU.mult), reads=[bH, c.b], writes=[bH])
            S.op("pool", lambda e, H_=H_: e.tensor_tensor(out=H_[:, 512:1024], in0=H_[:, 512:1024], in1=c.rb[:], op=ALU.add), reads=[bH, c.b], writes=[bH])
            S.op("dve", lambda e, H_=H_: e.tensor_tensor(out=H_[:], in0=H_[:], in1=gsig[:], op=ALU.mult), reads=[bH, b_gsig], writes=[bH])
            transpose_modulate(k, None, H_, bH, 0, zT, b_zT, 0, tpi, banks=(6,))
            xr_, bxr = xr[ci % 2], b_xr[ci % 2]
            ot_, bot = ot[ci % 2], b_ot[ci % 2]
            S.dma("sp", xr_[:], k.X[t * 128:(t + 1) * 128, :], reads=[k.Xb[t]], writes=[bxr])
            for h2 in range(2):
                for fc in range(8):
                    S.op("pe", lambda e, fc=fc, h2=h2: e.matmul(k.ps[7][:, :], lhsT=zT[:, fc, :], rhs=wo[:, fc, h2 * 512:(h2 + 1) * 512],
                                                               start=(fc == 0), stop=(fc == 7)),
                         reads=[b_zT, b_wo], writes=[k.psb[7]], inc=(fc == 7))
                pn_part(k, m, 0, 1.0, h2, k.ps[7][:, :], k.psb[7], ot_, bot)
            pn_finish(k, m, xr_, bxr, ot_, bot, small[ci % 2], b_small[ci % 2], k.X[t * 128:(t + 1) * 128, :], [k.Xb[t]])
        S.barrier()


_CACHE = {}


def _consts():
    c = {"ident": np.eye(128, dtype=np.float32)}
    perm = np.zeros((128, 128), np.float32)
    for d in range(128):
        if (d % 32) < 16:
            perm[d + 16, d] = -1.0
        else:
            perm[d - 16, d] = 1.0
    c["perm"] = perm
    kl = np.arange(128)[:, None]
    ql = np.arange(128)[None, :]
    c["maskp"] = np.tile((ql <= kl).astype(np.float32), (1, 4))
    c["maskn"] = np.tile((kl <= ql).astype(np.float32), (1, 4))
    t = np.arange(T)
    pos = np.stack([t // 64, t % 64], axis=0).astype(np.float32)
    inv = (10000.0 ** (-np.arange(16, dtype=np.float32) / 16.0)).astype(np.float32)
    d = np.arange(128) % 64
    ang = (pos[d // 32, :] * inv[d % 16][:, None]).astype(np.float32)
    c["ropec"] = np.cos(ang).astype(np.float32)
    c["ropes"] = np.sin(ang).astype(np.float32)
    jj = np.arange(128)
    tri0 = (jj[:, None] <= jj[None, :]).astype(np.float32)
    tri1 = (jj[:, None] >= jj[None, :]).astype(np.float32)
    c["tri"] = np.stack([tri0, tri1])
    c["mbm"] = -c["tri"]
    c["mwm"] = -(1.0 - c["tri"])
    c["mall"] = -np.ones((128, 128), np.float32)
    pos = np.zeros((128, 8), np.float32)
    L = 128
    pos[:, 0] = -(jj + 1)
    pos[:, 1] = -(L - jj)
    pos[:, 2] = jj + 1
    pos[:, 3] = L - jj
    pos[:, 4] = L - 1 - jj
    pos[:, 5] = jj
    c["posc"] = pos
    return c


def kernel(**inputs):
    stop_after = inputs.pop("_stop_after", None)
    f = lambda a: np.ascontiguousarray(np.asarray(a, dtype=np.float32))
    x = f(inputs["x"])
    B = x.shape[0]
    key = stop_after
    if key not in _CACHE:
        _CACHE[key] = build_nc(stop_after)
    nc = _CACHE[key]
    shared = {n: f(inputs[n]) for n in ("ada_w", "ada_b", "ln_g", "ln_b", "ffn_w_in", "ffn_w_out", "attn_w_in", "attn_w_out",
                                         "diff_lambda", "diff_norm_g", "sink_logits", "rec_w_in", "rec_w_out",
                                         "mlstm_gate_b", "mlstm_norm_g", "ret_decay_logit", "ret_norm_g", "ret_norm_b")}
    shared.update(_consts())
    c = f(inputs["c"])
    ctx = f(inputs["ctx"])
    c_ctx = f(inputs["c_ctx"])
    in_maps = []
    for b in range(B):
        d = dict(shared)
        d["x"] = x[b]
        d["ctx"] = ctx[b]
        d["cc"] = np.concatenate([c[b].reshape(8, 128), c_ctx.reshape(8, 128)], axis=0)
        in_maps.append(d)
    res = run_bass_kernel_spmd(nc, in_maps, core_ids=list(range(B)))
    kernel.last_dbg = res.results[0].get("dbg")
    if kernel.last_dbg is not None:
        np.save("last_dbg.npy", kernel.last_dbg)
    return np.stack([res.results[b]["out"] for b in range(B)], axis=0).astype(np.float32)
```
